# Optimizing a Trainium2 kernel written in Bass

```python
import jax
import jax.numpy as jnp
from jax import lax
import numpy as np

D_MODEL = 1024
BATCH = 32
SEQ = 2048
DEPTH = 1

CTX_LEN = 256
GRID_W = 64
EPS = 1e-6

ML_WIDTH = D_MODEL // 2
ML_HEADS = 4
ML_HEAD_DIM = ML_WIDTH // ML_HEADS
ML_CHUNK = 128
GLA_WIDTH = D_MODEL - ML_WIDTH
GLA_HEADS = 4
GLA_DV = GLA_WIDTH // GLA_HEADS
GLA_KEY_WIDTH = GLA_WIDTH // 2
GLA_DK = GLA_KEY_WIDTH // GLA_HEADS
GLA_RANK = 16
GLA_TAU = 16.0
GLA_CHUNK = 64
CONV_W = 3
MIX_WIDTH = ML_WIDTH + GLA_WIDTH
PROJ_WIDTHS = (ML_WIDTH, ML_WIDTH, ML_WIDTH, ML_WIDTH, 4 * ML_HEADS,
               GLA_KEY_WIDTH, GLA_KEY_WIDTH, GLA_WIDTH, GLA_WIDTH, 2 * GLA_RANK)
P_TOTAL = sum(PROJ_WIDTHS)

PEER_HEADS = 8
PEER_NKEYS = 128
PEER_EXPERTS = PEER_NKEYS * PEER_NKEYS
PEER_TOPK = 16
PEER_QDIM = 256
PEER_BLOCK = 128

kernel_name = 'hymba_mlstm_gla_peer_dit_block'


def rms_norm(x, g):
    xf = x.astype(jnp.float32)
    y = xf * lax.rsqrt(jnp.mean(xf * xf, axis=-1, keepdims=True) + EPS)
    return y.astype(x.dtype) * g


def head_rms(h, g):
    y = h * lax.rsqrt(jnp.mean(h * h, axis=-1, keepdims=True) + EPS)
    return y.reshape(h.shape[:2] + (-1,)) * g


def short_conv(u, w, on_grid):
    bz, n, ch = u.shape
    if on_grid:
        rows = n // GRID_W
        u = u.reshape(bz * rows, GRID_W, ch)
    y = lax.conv_general_dilated(u, w[:, None, :].astype(u.dtype), (1,),
                                 [(CONV_W // 2, CONV_W // 2)],
                                 dimension_numbers=('NWC', 'WIO', 'NWC'),
                                 feature_group_count=ch)
    return y.reshape(bz, n, ch)


def to_chunks(t, size):
    bz, n, hh = t.shape[:3]
    t = t.reshape((bz, n // size, size, hh) + t.shape[3:])
    return jnp.moveaxis(t, 3, 1)


def from_chunks(t):
    bz, hh, nc, ll, d = t.shape
    return jnp.moveaxis(t, 1, 3).reshape(bz, nc * ll, hh, d)


def orient(t, direction):
    return jnp.flip(t, axis=1) if direction == 1 else t


def prep_mixers(z, conv_w, gate_b, lr_w2, alpha_b, on_grid):
    bz, n, _ = z.shape
    f32 = jnp.float32
    mq, mk, mv, mo, mg, gq, gk, gv, gr, glr = jnp.split(
        z, np.cumsum(PROJ_WIDTHS)[:-1].tolist(), axis=-1)
    mq, mk = jnp.split(jax.nn.silu(short_conv(jnp.concatenate([mq, mk], axis=-1), conv_w, on_grid)),
                       2, axis=-1)
    gates = (mg + gate_b).astype(f32).reshape(bz, n, 2, 2, ML_HEADS)
    alpha = jnp.einsum('bndr,drk->bndk', glr.reshape(bz, n, 2, GLA_RANK), lr_w2) + alpha_b
    return {
        'ml_q': mq.astype(f32).reshape(bz, n, ML_HEADS, ML_HEAD_DIM),
        'ml_k': (mk.astype(f32) * ML_HEAD_DIM ** -0.5).reshape(bz, n, ML_HEADS, ML_HEAD_DIM),
        'ml_v': mv.astype(f32).reshape(bz, n, ML_HEADS, ML_HEAD_DIM),
        'ml_o': mo,
        'ml_ig': gates[:, :, :, 0],
        'ml_lf': jax.nn.log_sigmoid(gates[:, :, :, 1]),
        'gla_q': (gq.astype(f32) * GLA_DK ** -0.5).reshape(bz, n, GLA_HEADS, GLA_DK),
        'gla_k': gk.astype(f32).reshape(bz, n, GLA_HEADS, GLA_DK),
        'gla_v': gv.astype(f32).reshape(bz, n, GLA_HEADS, GLA_DV),
        'gla_r': gr,
        'gla_la': (jax.nn.log_sigmoid(alpha.astype(f32)) / GLA_TAU).reshape(
            bz, n, 2, GLA_HEADS, GLA_DK),
    }


def mlstm_states(k, v, logf, ig, init):
    b = jnp.cumsum(logf, axis=-1)
    b_end = b[..., -1]
    w_log = b_end[..., None] - b + ig
    m_loc = jnp.max(w_log, axis=-1)
    w = jnp.exp(w_log - m_loc[..., None])
    c_loc = jnp.einsum('bhcld,bhcle->bhcde', w[..., None] * v, k)
    n_loc = jnp.einsum('bhcl,bhcle->bhce', w, k)

    def step(carry, inp):
        cm, nv, m = carry
        be, ml, cl, nl = inp
        m_new = jnp.maximum(be + m, ml)
        a = jnp.exp(be + m - m_new)
        bb = jnp.exp(ml - m_new)
        c_new = a[..., None, None] * cm + bb[..., None, None] * cl
        n_new = a[..., None] * nv + bb[..., None] * nl
        return (c_new, n_new, m_new), (cm, nv, m)

    xs = tuple(jnp.moveaxis(t, 2, 0) for t in (b_end, m_loc, c_loc, n_loc))
    final, starts = lax.scan(step, init, xs)
    starts = tuple(jnp.moveaxis(t, 0, 2) for t in starts)
    return starts, final


def mlstm_outputs(q, k, v, logf, ig, starts):
    c_s, n_s, m_s = starts
    ll = q.shape[-2]
    tri = jnp.tril(jnp.ones((ll, ll), dtype=bool))
    b = jnp.cumsum(logf, axis=-1)
    d_log = jnp.where(tri, b[..., :, None] - b[..., None, :] + ig[..., None, :], -jnp.inf)
    inter_log = b + m_s[..., None]
    m_t = jnp.maximum(inter_log, jnp.max(d_log, axis=-1))
    scores = jnp.einsum('bhctd,bhcsd->bhcts', q, k) * jnp.exp(d_log - m_t[..., None])
    inter = jnp.exp(inter_log - m_t)
    num = (jnp.einsum('bhcts,bhcsd->bhctd', scores, v)
           + inter[..., None] * jnp.einsum('bhcde,bhcte->bhctd', c_s, q))
    den = scores.sum(-1) + inter * jnp.einsum('bhce,bhcte->bhct', n_s, q)
    return num / jnp.maximum(jnp.abs(den), jnp.exp(-m_t))[..., None]


def mlstm_direction(lat, ctx, with_ctx_out):
    q, k, v, lf, ig = lat
    qc, kc, vc, lfc, igc = ctx
    bz, hh, _, _, d = q.shape
    init = (jnp.zeros((bz, hh, d, d), jnp.float32), jnp.zeros((bz, hh, d), jnp.float32),
            jnp.zeros((bz, hh), jnp.float32))
    ctx_starts, ctx_final = mlstm_states(kc, vc, lfc, igc, init)
    lat_starts, _ = mlstm_states(k, v, lf, ig, ctx_final)
    h = mlstm_outputs(q, k, v, lf, ig, lat_starts)
    hc = mlstm_outputs(qc, kc, vc, lfc, igc, ctx_starts) if with_ctx_out else None
    return h, hc


def gla_states(k, v, loga, init):
    b = jnp.cumsum(loga, axis=-2)
    b_end = b[..., -1, :]
    s_loc = jnp.einsum('bhcld,bhcle->bhcde', k * jnp.exp(b_end[..., None, :] - b), v)

    def step(s, inp):
        be, sl = inp
        return jnp.exp(be)[..., None] * s + sl, s

    final, starts = lax.scan(step, init, (jnp.moveaxis(b_end, 2, 0), jnp.moveaxis(s_loc, 2, 0)))
    return jnp.moveaxis(starts, 0, 2), final


def gla_outputs(q, k, v, loga, s_start):
    ll = q.shape[-2]
    tri = jnp.tril(jnp.ones((ll, ll), dtype=bool))
    b = jnp.cumsum(loga, axis=-2)
    ref = b[..., ll // 2, :][..., None, :]
    att = jnp.einsum('bhctd,bhcsd->bhcts', q * jnp.exp(b - ref), k * jnp.exp(ref - b))
    att = jnp.where(tri, att, 0.0)
    return (jnp.einsum('bhcts,bhcse->bhcte', att, v)
            + jnp.einsum('bhctd,bhcde->bhcte', q * jnp.exp(b), s_start))


def gla_direction(lat, ctx, with_ctx_out):
    q, k, v, la = lat
    qc, kc, vc, lac = ctx
    bz, hh = q.shape[:2]
    init = jnp.zeros((bz, hh, GLA_DK, GLA_DV), jnp.float32)
    ctx_starts, ctx_final = gla_states(kc, vc, lac, init)
    lat_starts, _ = gla_states(k, v, la, ctx_final)
    h = gla_outputs(q, k, v, la, lat_starts)
    hc = gla_outputs(qc, kc, vc, lac, ctx_starts) if with_ctx_out else None
    return h, hc


def ml_inputs(p, d):
    return [to_chunks(orient(t, d), ML_CHUNK)
            for t in (p['ml_q'], p['ml_k'], p['ml_v'], p['ml_lf'][:, :, d], p['ml_ig'][:, :, d])]


def gla_inputs(p, d):
    return [to_chunks(orient(t, d), GLA_CHUNK)
            for t in (p['gla_q'], p['gla_k'], p['gla_v'], p['gla_la'][:, :, d])]


def mix_out(ml_h, ml_o, gla_h, gla_r, ml_g, gla_g, w_out, dtype):
    ml = jax.nn.sigmoid(ml_o) * head_rms(ml_h, ml_g)
    gl = jax.nn.silu(gla_r) * head_rms(gla_h, gla_g)
    return (jnp.concatenate([ml, gl], axis=-1) @ w_out).astype(dtype)


def token_mixer(h_lat, h_ctx, w_in, conv_w, gate_b, lr_w2, alpha_b, ml_g, gla_g, w_out,
                with_ctx_out):
    lat = prep_mixers(h_lat @ w_in, conv_w, gate_b, lr_w2, alpha_b, True)
    ctx = prep_mixers(h_ctx @ w_in, conv_w, gate_b, lr_w2, alpha_b, False)
    ml_lat, ml_ctx, gla_lat, gla_ctx = [], [], [], []
    for d in range(2):
        h, hc = mlstm_direction(ml_inputs(lat, d), ml_inputs(ctx, d), with_ctx_out)
        ml_lat.append(orient(from_chunks(h), d))
        g, gc = gla_direction(gla_inputs(lat, d), gla_inputs(ctx, d), with_ctx_out)
        gla_lat.append(orient(from_chunks(g), d))
        if with_ctx_out:
            ml_ctx.append(orient(from_chunks(hc), d))
            gla_ctx.append(orient(from_chunks(gc), d))
    y = mix_out(ml_lat[0] + ml_lat[1], lat['ml_o'], gla_lat[0] + gla_lat[1], lat['gla_r'],
                ml_g, gla_g, w_out, h_lat.dtype)
    yc = None
    if with_ctx_out:
        yc = mix_out(ml_ctx[0] + ml_ctx[1], ctx['ml_o'], gla_ctx[0] + gla_ctx[1], ctx['gla_r'],
                     ml_g, gla_g, w_out, h_ctx.dtype)
    return y, yc


def peer_ffn(h, wq, keys, u_tab, v_tab):
    bz, n, d = h.shape
    blocks = h.reshape(bz * n // PEER_BLOCK, PEER_BLOCK, d)

    def retrieve(xb):
        q = (xb @ wq).reshape(PEER_BLOCK, PEER_HEADS, 2, PEER_QDIM // 2)
        s = jnp.einsum('tphq,phkq->tphk', q, keys)
        top_s, top_i = lax.top_k(s, PEER_TOPK)
        cand = top_s[:, :, 0, :, None] + top_s[:, :, 1, None, :]
        best_s, best_c = lax.top_k(cand.reshape(PEER_BLOCK, PEER_HEADS, PEER_TOPK * PEER_TOPK),
                                   PEER_TOPK)
        i1 = jnp.take_along_axis(top_i[:, :, 0], best_c // PEER_TOPK, axis=-1)
        i2 = jnp.take_along_axis(top_i[:, :, 1], best_c % PEER_TOPK, axis=-1)
        idx = i1 * PEER_NKEYS + i2
        g = jax.nn.softmax(best_s.astype(jnp.float32), axis=-1).astype(xb.dtype)
        act = jax.nn.gelu(jnp.einsum('tpkd,td->tpk', u_tab[idx], xb), approximate=False)
        return jnp.einsum('tpk,tpkd->td', g * act, v_tab[idx])

    return lax.map(retrieve, blocks).reshape(bz, n, d)


def setup_inputs(seed: int = 0) -> dict:
    key = jax.random.key(seed)
    ks = jax.random.split(key, 24)

    def nrm(k, shape, scale):
        return jax.random.normal(k, shape, jnp.float32) * scale

    f_bias = jnp.linspace(3.0, 6.0, ML_HEADS, dtype=jnp.float32)
    i_part = nrm(ks[8], (DEPTH, 2, 1, ML_HEADS), 0.1)
    f_part = f_bias + nrm(ks[9], (DEPTH, 2, 1, ML_HEADS), 0.1)
    ml_gate_b = jnp.concatenate([i_part, f_part], axis=2).reshape(DEPTH, 4 * ML_HEADS)
    return {
        'x': nrm(ks[0], (BATCH, SEQ, D_MODEL), 1.0),
        'c': nrm(ks[1], (BATCH, D_MODEL), 1.0),
        'ctx': nrm(ks[2], (BATCH, CTX_LEN, D_MODEL), 1.0),
        'c_ctx': nrm(ks[3], (D_MODEL,), 1.0),
        'w_mod': nrm(ks[4], (DEPTH, D_MODEL, 6 * D_MODEL), 0.5 * D_MODEL ** -0.5),
        'b_mod': nrm(ks[5], (DEPTH, 6 * D_MODEL), 0.02),
        'norm1_g': 1.0 + nrm(ks[6], (DEPTH, D_MODEL), 0.02),
        'w_in': nrm(ks[7], (DEPTH, D_MODEL, P_TOTAL), D_MODEL ** -0.5),
        'ml_conv_w': nrm(ks[10], (DEPTH, CONV_W, 2 * ML_WIDTH), CONV_W ** -0.5),
        'ml_gate_b': ml_gate_b,
        'ml_norm_g': 1.0 + nrm(ks[11], (DEPTH, ML_WIDTH), 0.02),
        'gla_lr_w2': nrm(ks[12], (DEPTH, 2, GLA_RANK, GLA_KEY_WIDTH), GLA_RANK ** -0.5),
        'gla_alpha_b': nrm(ks[13], (DEPTH, 2, GLA_KEY_WIDTH), 0.1),
        'gla_norm_g': 1.0 + nrm(ks[14], (DEPTH, GLA_WIDTH), 0.02),
        'w_out': nrm(ks[15], (DEPTH, MIX_WIDTH, D_MODEL), MIX_WIDTH ** -0.5),
        'norm2_g': 1.0 + nrm(ks[16], (DEPTH, D_MODEL), 0.02),
        'peer_wq': nrm(ks[17], (DEPTH, D_MODEL, PEER_HEADS * PEER_QDIM), D_MODEL ** -0.5),
        'peer_keys': nrm(ks[18], (DEPTH, PEER_HEADS, 2, PEER_NKEYS, PEER_QDIM // 2),
                         (PEER_QDIM // 2) ** -0.5),
        'peer_u': nrm(ks[19], (DEPTH, PEER_EXPERTS, D_MODEL), D_MODEL ** -0.5),
        'peer_v': nrm(ks[20], (DEPTH, PEER_EXPERTS, D_MODEL), PEER_HEADS ** -0.5),
        'norm_f_g': 1.0 + nrm(ks[21], (D_MODEL,), 0.02),
    }


def reference(x, c, ctx, c_ctx, w_mod, b_mod, norm1_g, w_in, ml_conv_w, ml_gate_b, ml_norm_g,
              gla_lr_w2, gla_alpha_b, gla_norm_g, w_out, norm2_g, peer_wq, peer_keys, peer_u,
              peer_v, norm_f_g):
    cond = jax.nn.silu(c)
    cond_ctx = jax.nn.silu(c_ctx)
    for layer in range(DEPTH):
        update_ctx = layer + 1 < DEPTH
        mod = (cond @ w_mod[layer] + b_mod[layer])[:, None, :]
        mod_c = cond_ctx @ w_mod[layer] + b_mod[layer]
        sh1, sc1, g1, sh2, sc2, g2 = jnp.split(mod, 6, axis=-1)
        csh1, csc1, cg1, csh2, csc2, cg2 = jnp.split(mod_c, 6, axis=-1)

        h = rms_norm(x, norm1_g[layer]) * (1.0 + sc1) + sh1
        hc = rms_norm(ctx, norm1_g[layer]) * (1.0 + csc1) + csh1
        y, yc = token_mixer(h, hc, w_in[layer], ml_conv_w[layer], ml_gate_b[layer],
                            gla_lr_w2[layer], gla_alpha_b[layer], ml_norm_g[layer],
                            gla_norm_g[layer], w_out[layer], update_ctx)
        x = x + g1 * y
        h2 = rms_norm(x, norm2_g[layer]) * (1.0 + sc2) + sh2
        x = x + g2 * peer_ffn(h2, peer_wq[layer], peer_keys[layer], peer_u[layer], peer_v[layer])

        if update_ctx:
            ctx = ctx + cg1 * yc
            hc2 = rms_norm(ctx, norm2_g[layer]) * (1.0 + csc2) + csh2
            ctx = ctx + cg2 * peer_ffn(hc2, peer_wq[layer], peer_keys[layer], peer_u[layer],
                                       peer_v[layer])
    return rms_norm(x, norm_f_g)
```

```python
import math
from contextlib import ExitStack

import numpy as np
import concourse.bass as bass
import concourse.mybir as mybir
from concourse.bass_utils import run_bass_kernel_spmd

F32 = mybir.dt.float32
BF16 = mybir.dt.bfloat16
AF = mybir.ActivationFunctionType
ALU = mybir.AluOpType

D = 1024
EPS = 1e-6
NEG = -1.0e30
NEXP = 16384


class Buf:
    __slots__ = ("w", "r", "dsem", "dcnt")

    def __init__(s):
        s.w = None
        s.r = {}
        s.dsem = None
        s.dcnt = 0


class TB:
    def __init__(s, t):
        s.t = t
        s.b = Buf()

    def __getitem__(s, idx):
        return s.t[idx]


class KB:
    def __init__(s, nc, es):
        s.nc = nc
        s.es = es
        s.E = {'pe': nc.tensor, 'act': nc.scalar, 'dve': nc.vector, 'pool': nc.gpsimd, 'sp': nc.sync}
        s.sem = {}
        s.cnt = {}
        for k in s.E:
            s.sem[k] = es.enter_context(nc.semaphore("s_" + k))
            s.cnt[k] = 0
        s.waited = {k: {} for k in s.E}
        s.nsem = 0
        s.ninst = 0
        s.free = []
        s.swsems = set()
        s.dlast = {}

    def _deps(s, reads, writes):
        deps = {}
        for b in reads:
            b = b.b if isinstance(b, TB) else b
            if b.w is not None:
                k, v = b.w
                deps[k] = max(deps.get(k, 0), v)
        for b in writes:
            b = b.b if isinstance(b, TB) else b
            if b.w is not None:
                k, v = b.w
                deps[k] = max(deps.get(k, 0), v)
            for k, v in b.r.items():
                deps[k] = max(deps.get(k, 0), v)
        return deps

    def _need(s, eng, deps, skip_self=False):
        w = s.waited[eng]
        for k, v in deps.items():
            if skip_self and k == eng:
                continue
            if w.get(k, 0) < v:
                s.E[eng].wait_ge(s.sem[k], v)
                w[k] = v

    def _mark(s, me, reads, writes):
        k, v = me
        for b in reads:
            b = b.b if isinstance(b, TB) else b
            b.r[k] = max(b.r.get(k, 0), v)
        for b in writes:
            b = b.b if isinstance(b, TB) else b
            b.w = me
            b.r = {}

    def op(s, eng, fn, reads=(), writes=(), chain=False):
        s._need(eng, s._deps(reads, writes), skip_self=(chain and eng == 'pe'))
        ins = fn(s.E[eng])
        s.cnt[eng] += 1
        s.ninst += 1
        ins.then_inc(s.sem[eng], 1)
        s._mark((eng, s.cnt[eng]), reads, writes)
        return ins

    def dma(s, q, out_ap, in_ap, reads=(), writes=(), **kw):
        s._need(q, s._deps(reads, writes))
        tb = writes[0]
        tb = tb.b if isinstance(tb, TB) else tb
        if tb.dsem is None:
            if s.free and q != 'pool':
                tb.dsem, tb.dcnt = s.free.pop()
            else:
                if q == 'pool':
                    s.swsems.add("d%d" % s.nsem)
                key = "d%d" % s.nsem
                s.nsem += 1
                s.sem[key] = s.es.enter_context(s.nc.semaphore(key))
                tb.dsem = key
        ins = s.E[q].dma_start(out=out_ap, in_=in_ap, **kw)
        s.ninst += 1
        tb.dcnt += 16
        ins.then_inc(s.sem[tb.dsem], 16)
        s.dlast[tb.dsem] = tb.dcnt
        s._mark((tb.dsem, tb.dcnt), reads, writes)
        return ins

    def release(s, tb):
        b = tb.b if isinstance(tb, TB) else tb
        if b.dsem is not None:
            if b.dsem not in s.swsems:
                s.free.append((b.dsem, b.dcnt))
            b.dsem = None

    def wait_all(s, eng, bufs):
        s._need(eng, s._deps(bufs, ()))

    def barrier(s, dma_bufs=()):
        deps = {k: s.cnt[k] for k in s.E if s.cnt[k] > 0}
        deps.update(s.dlast)
        for e in s.E:
            s._need(e, dict(deps))


def build(NSEQ, N, C, dbg=False):
    NL = N // 128
    NCT = C // 128
    NT = NL + NCT
    T = N + C
    nc = bass.Bass("TRN2", target_bir_lowering=False)
    dt = lambda name, shape, dty=F32, kind="ExternalInput": nc.dram_tensor(name, shape, dty, kind=kind).ap()
    x_d = dt("x", [NSEQ, N, D])
    ctx_d = dt("ctx", [NSEQ, C, D])
    ccT_d = dt("ccT", [128, 8, NSEQ + 1])
    wmod_d = dt("w_mod", [D, 6 * D])
    bmodT_d = dt("bmodT", [128, 48])
    bmod_d = dt("b_mod", [1, 6 * D])
    ng_d = dt("ngT", [128, 2, 8])
    wfm_d = dt("w_fm", [D, 13 * 128])
    wtm_d = dt("w_tm", [D, 2064])
    cw_d = dt("cwT", [128, 8, 3])
    gb_d = dt("gate_b", [1, 16])
    mlg_d = dt("ml_g", [1, 512])
    glg_d = dt("gla_g", [1, 512])
    lrw_d = dt("lrw2", [64, 256])
    ab_d = dt("alpha_b", [1, 512])
    wout_d = dt("w_out", [D, D])
    wq_d = dt("wq", [D, 2048])
    keysT_d = dt("keysT", [128, 16 * 128])
    uT_d = dt("uT", [D, NEXP])
    v_d = dt("v", [NEXP, D])
    nf_d = dt("nf_g", [1, D])
    y_d = dt("y", [NSEQ, N, D], F32, "ExternalOutput")
    x1_d = dt("x1s", [NSEQ, N, D], F32, "ExternalOutput" if dbg else "Internal")
    uTb_d = dt("uTb", [D, NEXP], BF16, "Internal")
    vb_d = dt("vb", [NEXP, D], BF16, "Internal")

    with ExitStack() as es:
        kb = KB(nc, es)

        uid = [0]

        def sb(ctx, name, shape, dty=F32):
            uid[0] += 1
            tb = TB(ctx.enter_context(nc.sbuf_tensor("%s_t%d" % (name, uid[0]), shape, dty)))
            if ctx is not es:
                ctx.callback(kb.release, tb)
            return tb

        identF = sb(es, "identF", [128, 128])
        identB = sb(es, "identB", [128, 128], BF16)
        tri = [sb(es, "tri%d" % d, [128, 128]) for d in range(2)]
        ntri16 = [sb(es, "ntri%d" % d, [128, 128]) for d in range(2)]
        ptri16 = [sb(es, "ptri%d" % d, [128, 128]) for d in range(2)]
        ones1 = sb(es, "ones1", [1, 128])
        gbrep = sb(es, "gbrep", [128, 16])
        mlgrep = sb(es, "mlgrep", [128, 512])
        glgrep = sb(es, "glgrep", [128, 512])
        lrw = sb(es, "lrw", [64, 256])
        abrow = sb(es, "abrow", [1, 512])
        ngT = sb(es, "ngT", [128, 2, 8])
        cwT = sb(es, "cwT", [128, 8, 3])
        bmodT = sb(es, "bmodT", [128, 48])
        ccT = sb(es, "ccT", [128, 8, NSEQ + 1])
        epsc = sb(es, "epsc", [128, 1])
        onec = sb(es, "onec", [128, 1])
        lnks = sb(es, "lnks", [128, 1])
        lnqs = sb(es, "lnqs", [128, 1])
        lrwb = sb(es, "lrwb", [64, 256], BF16)
        ones1b = sb(es, "ones1b", [1, 128], BF16)
        abrowb = sb(es, "abrowb", [1, 512], BF16)
        psum = [TB(es.enter_context(nc.psum_tensor("pb%d" % i, [128, 512], F32))) for i in range(8)]

        def pbf(i):
            return psum[i].t[:].bitcast(BF16)

        kb.op('pool', lambda e: e.memset(identF[:], 0.0), writes=[identF])
        kb.op('pool', lambda e: e.affine_select(out=identF[:], in_=identF[:], pattern=[[-1, 128]], compare_op=ALU.not_equal,
                                                fill=1.0, base=0, channel_multiplier=1), reads=[identF], writes=[identF])
        kb.op('dve', lambda e: e.tensor_copy(out=identB[:], in_=identF[:]), reads=[identF], writes=[identB])
        for d in range(2):
            kb.op('pool', lambda e: e.memset(tri[d][:], 1.0), writes=[tri[d]])
            kb.op('pool', lambda e: e.affine_select(out=tri[d][:], in_=tri[d][:], pattern=[[1 if d == 0 else -1, 128]],
                                                    compare_op=ALU.is_ge, fill=0.0, base=0,
                                                    channel_multiplier=(-1 if d == 0 else 1)), reads=[tri[d]], writes=[tri[d]])
            kb.op('dve', lambda e: e.tensor_scalar(out=ntri16[d][:], in0=tri[d][:], scalar1=-1.0 / 16.0, scalar2=None, op0=ALU.mult),
                  reads=[tri[d]], writes=[ntri16[d]])
            kb.op('dve', lambda e: e.tensor_scalar(out=ptri16[d][:], in0=tri[d][:], scalar1=1.0 / 16.0, scalar2=None, op0=ALU.mult),
                  reads=[tri[d]], writes=[ptri16[d]])
        kb.op('pool', lambda e: e.memset(ones1[:], 1.0), writes=[ones1])
        kb.op('pool', lambda e: e.memset(ones1b[:], 1.0), writes=[ones1b])
        kb.op('pool', lambda e: e.memset(epsc[:], EPS), writes=[epsc])
        kb.op('pool', lambda e: e.memset(onec[:], 1.0), writes=[onec])
        kb.op('pool', lambda e: e.memset(lnks[:], -0.5 * math.log(128.0)), writes=[lnks])
        kb.op('pool', lambda e: e.memset(lnqs[:], math.log(0.125)), writes=[lnqs])
        kb.dma('sp', gbrep[:], gb_d.partition_broadcast(128), writes=[gbrep])
        kb.dma('sp', mlgrep[:], mlg_d.partition_broadcast(128), writes=[mlgrep])
        kb.dma('sp', glgrep[:], glg_d.partition_broadcast(128), writes=[glgrep])
        kb.dma('sp', lrw[:], lrw_d, writes=[lrw])
        kb.dma('sp', abrow[:], ab_d, writes=[abrow])
        kb.dma('sp', ngT[:], ng_d, writes=[ngT])
        kb.dma('sp', cwT[:], cw_d, writes=[cwT])
        kb.dma('sp', bmodT[:], bmodT_d, writes=[bmodT])
        kb.dma('sp', ccT[:], ccT_d, writes=[ccT])
        kb.op('dve', lambda e: e.tensor_copy(out=lrwb[:], in_=lrw[:]), reads=[lrw], writes=[lrwb])
        kb.op('dve', lambda e: e.tensor_copy(out=abrowb[:], in_=abrow[:]), reads=[abrow], writes=[abrowb])

        uTb_b = Buf()
        vb_b = Buf()
        x1_b = [[Buf()] * NL for _ in range(NSEQ)]
        y_b = Buf()

        with ExitStack() as ph:
            st = [sb(ph, "cst%d" % i, [128, 4096]) for i in range(2)]
            sb16 = [sb(ph, "csb%d" % i, [128, 4096], BF16) for i in range(2)]
            i = 0
            engs = ['act', 'dve']
            for (src, dst, dstb) in ((uT_d, uTb_d, uTb_b), (v_d, vb_d, vb_b)):
                rows, cols = src.shape
                sv = src.rearrange("(a p) c -> p a c", p=128)
                dv = dst.rearrange("(a p) c -> p a c", p=128)
                na = rows // 128
                cw = 4096 // 1
                for a in range(na):
                    for c0 in range(0, cols, 4096):
                        j = i % 2
                        w_ = min(4096, cols - c0)
                        kb.dma('sp', st[j][:, 0:w_], sv[:, a, c0:c0 + w_], writes=[st[j]])
                        eng = engs[i % 2]
                        if eng == 'act':
                            kb.op('act', lambda e: e.copy(out=sb16[j][:, 0:w_], in_=st[j][:, 0:w_]), reads=[st[j]], writes=[sb16[j]])
                        else:
                            kb.op(eng, lambda e: e.tensor_copy(out=sb16[j][:, 0:w_], in_=st[j][:, 0:w_]), reads=[st[j]], writes=[sb16[j]])
                        kb.dma('sp', dv[:, a, c0:c0 + w_], sb16[j][:, 0:w_], reads=[sb16[j]], writes=[dstb])
                        i += 1
        kb.barrier([uTb_b, vb_b])

        for b in range(NSEQ):
            with ExitStack() as sq:
                modT = sb(sq, "modT", [128, 48, 2])
                sc1 = sb(sq, "sc1", [128, 8, 2])
                sc2 = sb(sq, "sc2", [128, 8])
                g1rep = sb(sq, "grep", [128, D])
                g2rep = g1rep
                condT = sb(sq, "condT", [128, 8, 2], BF16)
                condrep = sb(sq, "condrep", [128, 8, 128], BF16)
                wbuf = [sb(sq, "wbuf%d" % i, [128, 8, 512], BF16) for i in range(2)]
                wcnt = [0]

                def load_w(src_ap, ncols):
                    j = wcnt[0] % 2
                    wcnt[0] += 1
                    kb.dma('pool', wbuf[j][:, :, 0:ncols], src_ap.rearrange("(k p) n -> p k n", p=128), writes=[wbuf[j]])
                    return wbuf[j]

                kb.op('act', lambda e: e.activation(out=condT[:, :, 0:1], in_=ccT[:, :, b:b + 1], func=AF.Silu), reads=[ccT], writes=[condT])
                kb.op('act', lambda e: e.activation(out=condT[:, :, 1:2], in_=ccT[:, :, NSEQ:NSEQ + 1], func=AF.Silu), reads=[ccT], writes=[condT])
                kb.op('dve', lambda e: e.tensor_copy(out=condrep[:], in_=condT[:, :, 0:1].broadcast_to([128, 8, 128])), reads=[condT], writes=[condrep])
                pm = psum[0]
                with ExitStack() as ph:
                    for g in range(12):
                        wb = load_w(wmod_d[:, g * 512:(g + 1) * 512], 512)
                        for c in range(4):
                            n = g * 4 + c
                            for k in range(8):
                                kb.op('pe', lambda e: e.matmul(pm[:, 2 * n:2 * n + 2], lhsT=wb[:, k, c * 128:(c + 1) * 128], rhs=condT[:, k, :],
                                                               start=(k == 0), stop=(k == 7)), reads=[wb, condT], writes=[pm], chain=(k > 0))
                    kb.op('dve', lambda e: e.tensor_tensor(out=modT[:], in0=pm[:, 0:96].rearrange("p (n r) -> p n r", r=2),
                                                           in1=bmodT[:].unsqueeze(2).broadcast_to([128, 48, 2]), op=ALU.add),
                          reads=[pm, bmodT], writes=[modT])
                    for r in range(2):
                        kb.op('dve', lambda e: e.scalar_tensor_tensor(out=sc1[:, :, r], in0=modT[:, 8:16, r], scalar=1.0, in1=ngT[:, 0, :],
                                                                      op0=ALU.add, op1=ALU.mult), reads=[modT, ngT], writes=[sc1])
                    kb.op('dve', lambda e: e.scalar_tensor_tensor(out=sc2[:], in0=modT[:, 32:40, 0], scalar=1.0, in1=ngT[:, 1, :],
                                                                  op0=ALU.add, op1=ALU.mult), reads=[modT, ngT], writes=[sc2])
                kb.barrier()

                def grep_build(ga, gb_):
                    with ExitStack() as ph:
                        brep = sb(ph, "brep", [128, 512])
                        for half, g in enumerate((ga, gb_)):
                            wb = load_w(wmod_d[:, g * 512:(g + 1) * 512], 512)
                            for k in range(8):
                                kb.op('pe', lambda e: e.matmul(psum[1][:], lhsT=condrep[:, k, :], rhs=wb[:, k, :], start=(k == 0), stop=(k == 7)),
                                      reads=[wb, condrep], writes=[psum[1]], chain=(k > 0))
                            kb.dma('sp', brep[:], bmod_d[:, g * 512:(g + 1) * 512].partition_broadcast(128), writes=[brep])
                            kb.op('dve', lambda e: e.tensor_tensor(out=g1rep[:, half * 512:(half + 1) * 512], in0=psum[1][:], in1=brep[:], op=ALU.add),
                                  reads=[psum[1], brep], writes=[g1rep])
                    kb.barrier()

                def norm_T(ph_tmp, src_ap, src_bufs, scale_col, bias_col, out_fn, out_buf, pidx=(0, 1)):
                    xt, junk, ss, xs = ph_tmp
                    kb.dma('sp', xt[:], src_ap, reads=src_bufs, writes=[xt])
                    kb.op('act', lambda e: e.activation(out=junk[:], in_=xt[:], func=AF.Square, accum_out=ss[:, 0:1]), reads=[xt], writes=[junk, ss])
                    kb.op('act', lambda e: e.activation(out=ss[:, 1:2], in_=ss[:, 0:1], func=AF.Ln, scale=1.0 / D, bias=epsc[:, 0:1]), reads=[ss, epsc], writes=[ss])
                    kb.op('act', lambda e: e.activation(out=ss[:, 2:3], in_=ss[:, 1:2], func=AF.Exp, scale=-0.5), reads=[ss], writes=[ss])
                    kb.op('dve', lambda e: e.tensor_scalar(out=xs[:], in0=xt[:], scalar1=ss[:, 2:3], scalar2=None, op0=ALU.mult), reads=[xt, ss], writes=[xs])
                    for hlf in range(2):
                        pb = psum[pidx[hlf]]
                        for kk in range(4):
                            k = hlf * 4 + kk
                            kb.op('pe', lambda e: e.transpose(pb[:, kk * 128:(kk + 1) * 128], xs[:, k * 128:(k + 1) * 128], identF[:]),
                                  reads=[xs, identF], writes=[pb])
                        for kk in range(4):
                            k = hlf * 4 + kk
                            eng = 'act' if kk % 2 == 0 else 'dve'
                            if eng == 'act':
                                kb.op('act', lambda e: e.activation(out=out_fn(k), in_=pb[:, kk * 128:(kk + 1) * 128], func=AF.Identity,
                                                                    scale=scale_col(k), bias=bias_col(k)), reads=[pb, modT, sc1, sc2], writes=[out_buf])
                            else:
                                kb.op('dve', lambda e: e.tensor_scalar(out=out_fn(k), in0=pb[:, kk * 128:(kk + 1) * 128], scalar1=scale_col(k),
                                                                       scalar2=bias_col(k), op0=ALU.mult, op1=ALU.add), reads=[pb, modT, sc1, sc2], writes=[out_buf])

                with ExitStack() as pm_:
                    hT = sb(pm_, "hT", [128, 8, T], BF16)
                    mixm = sb(pm_, "mixm", [128, NL, 512], BF16)
                    gates = sb(pm_, "gates", [128, NT, 16])
                    lfp = sb(pm_, "lfp", [128, NT, 8])
                    glrT = sb(pm_, "glrT", [128, T], BF16)
                    with ExitStack() as ph:
                        tmp = (sb(ph, "xt", [128, D]), sb(ph, "junk", [128, D]), sb(ph, "ss", [128, 4]), sb(ph, "xs", [128, D]))
                        for j in range(NT):
                            isctx = j < NCT
                            r = 1 if isctx else 0
                            src = ctx_d[b, j * 128:(j + 1) * 128, :] if isctx else x_d[b, (j - NCT) * 128:(j - NCT + 1) * 128, :]
                            norm_T(tmp, src, [], lambda k: sc1[:, k, r:r + 1], lambda k: modT[:, k, r:r + 1],
                                   lambda k: hT[:, k, j * 128:(j + 1) * 128], hT)
                    kb.barrier()

                    groups = [(g0, min(512, T - g0)) for g0 in range(0, T, 512)]

                    def proj_fm(ph, wb, coff, dst_fn, dst_buf, conv_k=None, zraw=None, zc=None):
                        for gi, (g0, gl) in enumerate(groups):
                            pb = psum[2 + gi % 2]
                            for k in range(8):
                                kb.op('pe', lambda e: e.matmul(pb[:, 0:gl], lhsT=wb[:, k, coff:coff + 128], rhs=hT[:, k, g0:g0 + gl],
                                                               start=(k == 0), stop=(k == 7)), reads=[wb, hT], writes=[pb], chain=(k > 0))
                            if conv_k is None:
                                kb.op('act', lambda e: e.copy(out=dst_fn(g0, gl), in_=pb[:, 0:gl]), reads=[pb], writes=[dst_buf])
                            else:
                                kb.op('act', lambda e: e.copy(out=zraw[:, g0:g0 + gl], in_=pb[:, 0:gl]), reads=[pb], writes=[zraw])
                        if conv_k is not None:
                            w0 = cwT[:, conv_k, 0:1]
                            w1 = cwT[:, conv_k, 1:2]
                            w2 = cwT[:, conv_k, 2:3]
                            kb.op('dve', lambda e: e.tensor_scalar(out=zc[:], in0=zraw[:], scalar1=w1, scalar2=None, op0=ALU.mult), reads=[zraw, cwT], writes=[zc])
                            regions = [(0, 1, C), (C, N // 64, 64)]
                            for (r0, nr, rl) in regions:
                                zr = zraw[:, r0:r0 + nr * rl].rearrange("p (a b) -> p a b", b=rl)
                                yr = zc[:, r0:r0 + nr * rl].rearrange("p (a b) -> p a b", b=rl)
                                kb.op('dve', lambda e: e.scalar_tensor_tensor(out=yr[:, :, 1:rl], in0=zr[:, :, 0:rl - 1], scalar=w0, in1=yr[:, :, 1:rl],
                                                                              op0=ALU.mult, op1=ALU.add), reads=[zraw, cwT, zc], writes=[zc])
                                kb.op('dve', lambda e: e.scalar_tensor_tensor(out=yr[:, :, 0:rl - 1], in0=zr[:, :, 1:rl], scalar=w2, in1=yr[:, :, 0:rl - 1],
                                                                              op0=ALU.mult, op1=ALU.add), reads=[zraw, cwT, zc], writes=[zc])
                            kb.op('act', lambda e: e.activation(out=dst_fn(0, T), in_=zc[:], func=AF.Silu), reads=[zc], writes=[dst_buf])

                    def proj_tm(wb, ncols, j, evac):
                        pb = psum[4 + j % 2]
                        for k in range(8):
                            kb.op('pe', lambda e: e.matmul(pb[:, 0:ncols], lhsT=hT[:, k, j * 128:(j + 1) * 128], rhs=wb[:, k, 0:ncols],
                                                           start=(k == 0), stop=(k == 7)), reads=[wb, hT], writes=[pb], chain=(k > 0))
                        evac(pb)

                    with ExitStack() as ph:
                        wb = load_w(wfm_d[:, 12 * 128:13 * 128], 128)
                        proj_fm(ph, wb, 0, lambda g0, gl: glrT[:, g0:g0 + gl], glrT)
                    kb.barrier()

                    def scan_pass(kind):
                        with ExitStack() as ar:
                            nq = 4 if kind == 0 else 2
                            qT = sb(ar, "qT", [128, nq, T], BF16)
                            kT = sb(ar, "kT", [128, nq, T], BF16)
                            vw = 129 if kind == 0 else 128
                            vt = sb(ar, "vt", [128, NT, 4, vw], BF16)
                            og = sb(ar, "og", [128, NL, 512], BF16)
                            with ExitStack() as ph:
                                zraw = sb(ph, "zraw", [128, T])
                                zc = sb(ph, "zc", [128, T])
                                if kind == 0:
                                    for half, dst in ((0, qT), (1, kT)):
                                        wb = load_w(wfm_d[:, half * 512:(half + 1) * 512], 512)
                                        for c in range(4):
                                            proj_fm(ph, wb, c * 128, (lambda g0, gl, c=c, dst=dst: dst[:, c, g0:g0 + gl]), dst,
                                                    conv_k=half * 4 + c, zraw=zraw, zc=zc)
                                    kb.op('pool', lambda e: e.memset(vt[:, :, :, 128:129], 1.0), writes=[vt])
                                    voff, ooff, ofn = 0, 512, AF.Sigmoid
                                else:
                                    wb = load_w(wfm_d[:, 1024:1536], 512)
                                    for c in range(4):
                                        dst = qT if c < 2 else kT
                                        proj_fm(ph, wb, c * 128, (lambda g0, gl, c=c, dst=dst: dst[:, c % 2, g0:g0 + gl]), dst)
                                    voff, ooff, ofn = 1024, 1536, AF.Silu
                                wb = load_w(wtm_d[:, voff:voff + 512], 512)
                                for j in range(NT):
                                    proj_tm(wb, 512, j, lambda pb: kb.op('act', lambda e: e.copy(out=vt[:, j, :, 0:128], in_=pb[:].rearrange("p (h e) -> p h e", e=128)),
                                                                           reads=[pb], writes=[vt]))
                                wb = load_w(wtm_d[:, ooff:ooff + 512], 512)
                                for j in range(NCT, NT):
                                    proj_tm(wb, 512, j, lambda pb: kb.op('act', lambda e: e.activation(out=og[:, j - NCT, :], in_=pb[:], func=ofn),
                                                                           reads=[pb], writes=[og]))
                                if kind == 0:
                                    wb = load_w(wtm_d[:, 2048:2064], 16)
                                    for j in range(NT):
                                        proj_tm(wb, 16, j, lambda pb: kb.op('dve', lambda e: e.tensor_tensor(out=gates[:, j, :], in0=pb[:, 0:16], in1=gbrep[:], op=ALU.add),
                                                                              reads=[pb, gbrep], writes=[gates]))
                                    gv4 = gates[:].rearrange("p j (d i h) -> p j d i h", d=2, i=2)
                                    for d in range(2):
                                        kb.op('act', lambda e: e.activation(out=lfp[:, :, d * 4:(d + 1) * 4], in_=gv4[:, :, d, 1, :], func=AF.Exp, scale=-1.0),
                                              reads=[gates], writes=[lfp])
                                    kb.op('act', lambda e: e.activation(out=lfp[:], in_=lfp[:], func=AF.Ln, bias=onec[:, 0:1]), reads=[lfp, onec], writes=[lfp])
                            kb.barrier()
                            with ExitStack() as ph:
                                S = sb(ph, "S", [128, 4, vw]) if kind == 0 else sb(ph, "S", [128, 2, 128])
                                Hb = sb(ph, "Hb", [128, NL, 512], BF16)
                                Sd = sb(ph, "Sd", S.t.shape)
                                Sbf = sb(ph, "Sbf", S.t.shape, BF16)
                                rep = [sb(ph, "rep%d" % i, [128, 4, 128]) for i in range(3)]
                                EB = sb(ph, "EB", [128, 4, 128])
                                EG = sb(ph, "EG", [128, 4, 128])
                                qt = sb(ph, "qt", [128, 4, 128], BF16)
                                kt = sb(ph, "kt", [128, 4, 128], BF16)
                                ktok = sb(ph, "ktok", [128, 4, 128], BF16)
                                PT = sb(ph, "PT", [128, 4, 128], BF16)
                                la = sb(ph, "la", [128, 256])
                                dec = sb(ph, "dec", [128, 4])
                                den = sb(ph, "den", [128, 8])
                                hn = sb(ph, "hn", [128, 4, 128])
                                ht = sb(ph, "ht", [128, 4, 128])
                                junk2 = sb(ph, "junk2", [128, 128])
                                hss = sb(ph, "hss", [128, 12])
                                if kind == 1:
                                    mix = sb(ph, "mix", [128, D], BF16)
                                    mixT = sb(ph, "mixT", [128, 8, 128], BF16)
                                    xres = sb(ph, "xres", [128, D])
                                    ytmp = sb(ph, "ytmp", [128, D])
                                    grep_build(4, 5)
                                    for hf_ in range(2):
                                        kb.dma('pool', wbuf[hf_][:], wout_d[:, hf_ * 512:(hf_ + 1) * 512].rearrange("(k p) n -> p k n", p=128), writes=[wbuf[hf_]])
                                grep_ = mlgrep if kind == 0 else glgrep
                                nh = 4
                                for d in (1, 0):
                                    order = list(range(NT)) if d == 0 else (list(range(NCT - 1, -1, -1)) + list(range(NT - 1, NCT - 1, -1)))
                                    last = 127 if d == 0 else 0
                                    kb.op('pool', lambda e: e.memset(S[:], 0.0), writes=[S])
                                    kb.op('pool', lambda e: e.memset(Sbf[:], 0.0), writes=[Sbf])
                                    for j in order:
                                        lat = j >= NCT
                                        jl = j - NCT
                                        tok = slice(j * 128, (j + 1) * 128)
                                        if kind == 0:
                                            lsrc = lfp[:, j, d * 4:(d + 1) * 4].unsqueeze(2).broadcast_to([128, 4, 128])
                                            isrc = gates[:, j, d * 8:d * 8 + 4].unsqueeze(2).broadcast_to([128, 4, 128])
                                            kb.op('dve', lambda e: e.tensor_scalar(out=rep[0][:], in0=lsrc, scalar1=-1.0, scalar2=None, op0=ALU.mult), reads=[lfp], writes=[rep[0]])
                                            kb.op('pool', lambda e: e.tensor_copy(out=rep[1][:], in_=lsrc), reads=[lfp], writes=[rep[1]])
                                            kb.op('pool', lambda e: e.tensor_copy(out=rep[2][:], in_=isrc), reads=[gates], writes=[rep[2]])
                                            for h in range(4):
                                                kb.op('pe', lambda e: e.matmul(psum[0][:, h * 128:(h + 1) * 128], lhsT=rep[0][:, h, :], rhs=tri[d][:], start=True, stop=True),
                                                      reads=[rep[0], tri[d]], writes=[psum[0]])
                                                kb.op('pe', lambda e: e.matmul(psum[1][:, h * 128:(h + 1) * 128], lhsT=rep[2][:, h, :], rhs=identF[:], start=True, stop=False),
                                                      reads=[rep[2], identF], writes=[psum[1]])
                                                kb.op('pe', lambda e: e.matmul(psum[1][:, h * 128:(h + 1) * 128], lhsT=rep[1][:, h, :], rhs=tri[d][:], start=False, stop=True),
                                                      reads=[rep[1], tri[d]], writes=[psum[1]])
                                            kb.op('act', lambda e: e.activation(out=EB[:].rearrange("p h t -> p (h t)"), in_=psum[0][:], func=AF.Exp), reads=[psum[0]], writes=[EB])
                                            kb.op('act', lambda e: e.activation(out=EG[:].rearrange("p h t -> p (h t)"), in_=psum[1][:], func=AF.Exp, bias=lnks[:, 0:1]),
                                                  reads=[psum[1], lnks], writes=[EG])
                                            kb.op('act', lambda e: e.copy(out=dec[:], in_=EB[:, :, last]), reads=[EB], writes=[dec])
                                            nb = 4
                                        else:
                                            kb.op('pe', lambda e: e.matmul(psum[0][:, 0:256], lhsT=glrT[32 * d:32 * d + 16, tok], rhs=lrwb[32 * d:32 * d + 16, :], start=True, stop=False),
                                                  reads=[glrT, lrwb], writes=[psum[0]])
                                            kb.op('pe', lambda e: e.matmul(psum[0][:, 0:256], lhsT=ones1b[:, :], rhs=abrowb[:, d * 256:(d + 1) * 256], start=False, stop=True),
                                                  reads=[ones1b, abrowb], writes=[psum[0]])
                                            kb.op('act', lambda e: e.activation(out=la[:], in_=psum[0][:, 0:256], func=AF.Exp, scale=-1.0), reads=[psum[0]], writes=[la])
                                            kb.op('act', lambda e: e.activation(out=la[:], in_=la[:], func=AF.Ln, bias=onec[:, 0:1]), reads=[la, onec], writes=[la])
                                            for c in range(2):
                                                kb.op('pe', lambda e: e.matmul(psum[1][:, c * 128:(c + 1) * 128], lhsT=la[:, c * 128:(c + 1) * 128], rhs=ntri16[d][:], start=True, stop=True),
                                                      reads=[la, ntri16[d]], writes=[psum[1]])
                                                kb.op('pe', lambda e: e.matmul(psum[1][:, 256 + c * 128:256 + (c + 1) * 128], lhsT=la[:, c * 128:(c + 1) * 128], rhs=ptri16[d][:], start=True, stop=True),
                                                      reads=[la, ptri16[d]], writes=[psum[1]])
                                            kb.op('act', lambda e: e.activation(out=EB[:, 0:2, :].rearrange("p h t -> p (h t)"), in_=psum[1][:, 0:256], func=AF.Exp, bias=lnqs[:, 0:1]),
                                                  reads=[psum[1], lnqs], writes=[EB])
                                            kb.op('act', lambda e: e.activation(out=EG[:, 0:2, :].rearrange("p h t -> p (h t)"), in_=psum[1][:, 256:512], func=AF.Exp), reads=[psum[1]], writes=[EG])
                                            kb.op('act', lambda e: e.activation(out=dec[:, 0:2], in_=psum[1][:, 0:256].rearrange("p (c t) -> p c t", t=128)[:, :, last], func=AF.Exp),
                                                  reads=[psum[1]], writes=[dec])
                                            nb = 2
                                        if lat:
                                            kb.op('dve', lambda e: e.tensor_tensor(out=qt[:, 0:nb, :], in0=qT[:, :, tok], in1=EB[:, 0:nb, :], op=ALU.mult), reads=[qT, EB], writes=[qt])
                                        kb.op('pool', lambda e: e.tensor_tensor(out=kt[:, 0:nb, :], in0=kT[:, :, tok], in1=EG[:, 0:nb, :], op=ALU.mult), reads=[kT, EG], writes=[kt])
                                        p2 = pbf(2)
                                        for c in range(nb):
                                            kb.op('pe', lambda e: e.transpose(p2[:, c * 128:(c + 1) * 128], kt[:, c, :], identB[:]), reads=[kt, identB], writes=[psum[2]])
                                        kb.op('act', lambda e: e.copy(out=ktok[:, 0:nb, :].rearrange("p h t -> p (h t)"), in_=p2[:, 0:nb * 128]), reads=[psum[2]], writes=[ktok])
                                        if lat:
                                            for h in range(4):
                                                if kind == 0:
                                                    l_, r_ = kt[:, h, :], qt[:, h, :]
                                                else:
                                                    c, hf = h // 2, h % 2
                                                    l_, r_ = kt[64 * hf:64 * hf + 64, c, :], qt[64 * hf:64 * hf + 64, c, :]
                                                if kind == 0:
                                                    sbk, scol = psum[3], h * 128
                                                else:
                                                    sbk, scol = (psum[3], psum[7])[h % 2], (h // 2) * 128
                                                kb.op('pe', lambda e: e.matmul(sbk[:, scol:scol + 128], lhsT=l_, rhs=r_, start=True, stop=True), reads=[kt, qt], writes=[sbk])
                                            if kind == 0:
                                                kb.op('dve', lambda e: e.tensor_tensor(out=PT[:], in0=psum[3][:].rearrange("p (h t) -> p h t", t=128),
                                                                                       in1=tri[d][:].unsqueeze(1).broadcast_to([128, 4, 128]), op=ALU.mult),
                                                      reads=[psum[3], tri[d]], writes=[PT])
                                            else:
                                                for hf in range(2):
                                                    sbk = (psum[3], psum[7])[hf]
                                                    kb.op('dve', lambda e: e.tensor_tensor(out=PT[:].rearrange("p (c f) t -> p c f t", f=2)[:, :, hf, :],
                                                                                           in0=sbk[:, 0:256].rearrange("p (c t) -> p c t", t=128),
                                                                                           in1=tri[d][:].unsqueeze(1).broadcast_to([128, 2, 128]), op=ALU.mult),
                                                          reads=[sbk, tri[d]], writes=[PT])
                                            for h in range(4):
                                                if kind == 0:
                                                    ob = psum[4 + h // 2]
                                                    oap = ob[:, (h % 2) * 129:(h % 2) * 129 + 129]
                                                    ql, sr = qt[:, h, :], Sbf[:, h, :]
                                                else:
                                                    c, hf = h // 2, h % 2
                                                    ob = psum[4 + hf]
                                                    oap = ob[:, c * 128:(c + 1) * 128]
                                                    ql, sr = qt[64 * hf:64 * hf + 64, c, :], Sbf[64 * hf:64 * hf + 64, c, :]
                                                kb.op('pe', lambda e: e.matmul(oap, lhsT=PT[:, h, :], rhs=vt[:, j, h, :], start=True, stop=False), reads=[PT, vt], writes=[ob])
                                                kb.op('pe', lambda e: e.matmul(oap, lhsT=ql, rhs=sr, start=False, stop=True), reads=[qt, Sbf], writes=[ob])
                                            if kind == 0:
                                                for bb in range(2):
                                                    o3 = psum[4 + bb][:, 0:258].rearrange("p (h e) -> p h e", e=129)
                                                    kb.op('act', lambda e: e.activation(out=den[:, bb * 2:bb * 2 + 2], in_=o3[:, :, 128], func=AF.Abs),
                                                          reads=[psum[4 + bb]], writes=[den])
                                                kb.op('dve', lambda e: e.tensor_scalar(out=den[:, 0:4], in0=den[:, 0:4], scalar1=1.0, scalar2=None, op0=ALU.max), reads=[den], writes=[den])
                                                kb.op('dve', lambda e: e.reciprocal(out=den[:, 4:8], in_=den[:, 0:4]), reads=[den], writes=[den])
                                                for bb in range(2):
                                                    o3 = psum[4 + bb][:, 0:258].rearrange("p (h e) -> p h e", e=129)
                                                    kb.op('dve', lambda e: e.tensor_tensor(out=hn[:, bb * 2:bb * 2 + 2, :], in0=o3[:, :, 0:128],
                                                                                           in1=den[:, 4 + bb * 2:6 + bb * 2].unsqueeze(2).broadcast_to([128, 2, 128]), op=ALU.mult),
                                                          reads=[psum[4 + bb], den], writes=[hn])
                                                hsrc = hn[:].rearrange("p h e -> p (h e)")
                                                hsrc_b = [hn]
                                            else:
                                                for hf in range(2):
                                                    kb.op('act', lambda e: e.copy(out=hn[:].rearrange("p (c f) e -> p c f e", f=2)[:, :, hf, :],
                                                                                  in_=psum[4 + hf][:, 0:256].rearrange("p (c e) -> p c e", e=128)),
                                                          reads=[psum[4 + hf]], writes=[hn])
                                                hsrc = hn[:].rearrange("p h e -> p (h e)")
                                                hsrc_b = [hn]
                                            if d == 1:
                                                kb.op('act', lambda e: e.copy(out=Hb[:, jl, :], in_=hsrc), reads=hsrc_b, writes=[Hb])
                                            else:
                                                kb.op('dve', lambda e: e.tensor_tensor(out=ht[:].rearrange("p h e -> p (h e)"), in0=hsrc, in1=Hb[:, jl, :], op=ALU.add),
                                                      reads=hsrc_b + [Hb], writes=[ht])
                                                for h in range(4):
                                                    kb.op('act', lambda e: e.activation(out=junk2[:], in_=ht[:, h, :], func=AF.Square, accum_out=hss[:, h:h + 1]),
                                                          reads=[ht], writes=[junk2, hss])
                                                kb.op('act', lambda e: e.activation(out=hss[:, 4:8], in_=hss[:, 0:4], func=AF.Ln, scale=1.0 / 128.0, bias=epsc[:, 0:1]),
                                                      reads=[hss, epsc], writes=[hss])
                                                kb.op('act', lambda e: e.activation(out=hss[:, 8:12], in_=hss[:, 4:8], func=AF.Exp, scale=-0.5), reads=[hss], writes=[hss])
                                                kb.op('dve', lambda e: e.tensor_tensor(out=ht[:], in0=ht[:], in1=hss[:, 8:12].unsqueeze(2).broadcast_to([128, 4, 128]), op=ALU.mult),
                                                      reads=[ht, hss], writes=[ht])
                                                kb.op('pool', lambda e: e.tensor_tensor(out=ht[:].rearrange("p h e -> p (h e)"), in0=ht[:].rearrange("p h e -> p (h e)"), in1=grep_[:], op=ALU.mult),
                                                      reads=[ht, grep_], writes=[ht])
                                                if kind == 0:
                                                    kb.op('dve', lambda e: e.tensor_tensor(out=mixm[:, jl, :], in0=ht[:].rearrange("p h e -> p (h e)"), in1=og[:, jl, :], op=ALU.mult),
                                                          reads=[ht, og], writes=[mixm])
                                                else:
                                                    kb.op('dve', lambda e: e.tensor_tensor(out=mix[:, 512:1024], in0=ht[:].rearrange("p h e -> p (h e)"), in1=og[:, jl, :], op=ALU.mult),
                                                          reads=[ht, og], writes=[mix])
                                                    kb.op('pool', lambda e: e.tensor_copy(out=mix[:, 0:512], in_=mixm[:, jl, :]), reads=[mixm], writes=[mix])
                                                    p6 = pbf(6)
                                                    for k in range(8):
                                                        kb.op('pe', lambda e: e.transpose(p6[:, k * 128:(k + 1) * 128], mix[:, k * 128:(k + 1) * 128], identB[:]),
                                                              reads=[mix, identB], writes=[psum[6]])
                                                    kb.op('act', lambda e: e.copy(out=mixT[:].rearrange("p k t -> p (k t)"), in_=p6[:, 0:1024]), reads=[psum[6]], writes=[mixT])
                                                    kb.dma('sp', xres[:], x_d[b, jl * 128:(jl + 1) * 128, :], writes=[xres])
                                                    for hf in range(2):
                                                        for k in range(8):
                                                            kb.op('pe', lambda e: e.matmul(psum[7][:], lhsT=mixT[:, k, :], rhs=wbuf[hf][:, k, :], start=(k == 0), stop=(k == 7)),
                                                                  reads=[mixT, wbuf[hf]], writes=[psum[7]], chain=(k > 0))
                                                        kb.op('dve', lambda e: e.tensor_tensor(out=ytmp[:, hf * 512:(hf + 1) * 512], in0=psum[7][:], in1=g1rep[:, hf * 512:(hf + 1) * 512], op=ALU.mult),
                                                              reads=[psum[7], g1rep], writes=[ytmp])
                                                    kb.op('pool', lambda e: e.tensor_tensor(out=ytmp[:], in0=ytmp[:], in1=xres[:], op=ALU.add), reads=[ytmp, xres], writes=[ytmp])
                                                    kb.dma('sp', x1_d[b, jl * 128:(jl + 1) * 128, :], ytmp[:], reads=[ytmp], writes=[x1_b[b][jl]])
                                        if kind == 0:
                                            for h in range(4):
                                                ub = psum[(0, 1)[h // 2]] if False else psum[6 + h // 2] if kind == 0 else None
                                                kb.op('pe', lambda e: e.matmul(ub[:, (h % 2) * 129:(h % 2) * 129 + 129], lhsT=ktok[:, h, :], rhs=vt[:, j, h, :], start=True, stop=True),
                                                      reads=[ktok, vt], writes=[ub])
                                            for bb in range(2):
                                                u3 = psum[6 + bb][:, 0:258].rearrange("p (h e) -> p h e", e=129)
                                                kb.op('dve', lambda e: e.tensor_tensor(out=Sd[:, bb * 2:bb * 2 + 2, :], in0=u3, in1=S[:, bb * 2:bb * 2 + 2, :], op=ALU.add),
                                                      reads=[psum[6 + bb], S], writes=[Sd])
                                            kb.op('pool', lambda e: e.tensor_tensor(out=S[:], in0=Sd[:], in1=dec[:].unsqueeze(2).broadcast_to([128, 4, vw]), op=ALU.mult),
                                                  reads=[Sd, dec], writes=[S])
                                        else:
                                            for h in range(4):
                                                c, hf = h // 2, h % 2
                                                kb.op('pe', lambda e: e.matmul(psum[5][64 * hf:64 * hf + 64, c * 128:(c + 1) * 128], lhsT=ktok[:, c, 64 * hf:64 * hf + 64], rhs=vt[:, j, h, :],
                                                                               start=True, stop=True), reads=[ktok, vt], writes=[psum[5]])
                                            kb.op('dve', lambda e: e.tensor_tensor(out=Sd[:], in0=psum[5][:, 0:256].rearrange("p (c e) -> p c e", e=128), in1=S[:], op=ALU.add),
                                                  reads=[psum[5], S], writes=[Sd])
                                            kb.op('pool', lambda e: e.tensor_tensor(out=S[:], in0=Sd[:], in1=dec[:, 0:2].unsqueeze(2).broadcast_to([128, 2, 128]), op=ALU.mult),
                                                  reads=[Sd, dec], writes=[S])
                                        kb.op('act', lambda e: e.copy(out=Sbf[:], in_=S[:]), reads=[S], writes=[Sbf])
                            kb.barrier()

                    scan_pass(0)
                    scan_pass(1)
                kb.barrier([x1_b[b][jl] for jl in range(NL)])

                with ExitStack() as pp:
                    NB3 = 3
                    xt3 = [sb(pp, "xt", [128, D]) for _ in range(NB3)]
                    h2T3 = [sb(pp, "h2T", [128, 8, 128], BF16) for _ in range(NB3)]
                    junk = sb(pp, "junk", [128, D], BF16)
                    xs = sb(pp, "xs", [128, D])
                    ss2 = [sb(pp, "ss", [128, 4]) for _ in range(2)]
                    nfrep = sb(pp, "nfrep", [128, D])
                    keysT = sb(pp, "keysT", [128, 16 * 128], BF16)
                    kb.dma('sp', nfrep[:], nf_d.partition_broadcast(128), writes=[nfrep])
                    kb.dma('pool', keysT[:], keysT_d, writes=[keysT])
                    grep_build(10, 11)
                    qTp = sb(pp, "qTp", [128, 16, 128], BF16)
                    sc2_ = [sb(pp, "sc", [128, 16, 128]) for _ in range(2)]
                    s2m = [sb(pp, "s2m", [128, 8, 128]) for _ in range(2)]
                    work = sb(pp, "work", [128, 256])
                    tv = sb(pp, "tv", [128, 16, 16])
                    cand = sb(pp, "cand", [128, 16, 16])
                    c162 = [sb(pp, "c16", [128, 8, 16]) for _ in range(2)]
                    e16 = sb(pp, "e16", [128, 16])
                    st82 = [sb(pp, "st8", [128, 5, 8]) for _ in range(2)]
                    W = sb(pp, "W", [128, NEXP], BF16)
                    Wb = [Buf() for _ in range(8)]
                    Sq = [sb(pp, "Sq", [128, 16, 128]) for _ in range(3)]
                    Eq = [sb(pp, "Eq", [128, 16, 128], BF16) for _ in range(3)]
                    ub = [sb(pp, "ub", [128, 8, 512], BF16) for _ in range(2)]
                    vbf = [sb(pp, "vbf", [128, 4, D], BF16) for _ in range(2)]
                    Gt = [sb(pp, "Gt", [128, 512], BF16) for _ in range(2)]
                    Xt = [sb(pp, "Xt", [128, 512], BF16) for _ in range(2)]
                    XT = [sb(pp, "XT", [128, 4, 128], BF16) for _ in range(2)]
                    x2 = xs
                    NEG_ = NEXP // 512

                    def stageA(i):
                        s3, s2_ = i % NB3, i % 2
                        xt, h2T, sc, c16, st8 = xt3[s3], h2T3[s3], sc2_[s2_], c162[s2_], st82[s2_]
                        tsl = slice(i * 128, (i + 1) * 128)
                        ss = ss2[0]
                        wbs = {}
                        steps = []

                        def s_load():
                            kb.dma('pool', xt[:], x1_d[b, tsl, :], reads=[x1_b[b][i]], writes=[xt])
                            wbs[0] = load_w(wq_d[:, 0:512], 512)
                        steps.append(s_load)
                        steps.append(lambda: None)

                        def s_stat():
                            kb.op('act', lambda e: e.activation(out=junk[:], in_=xt[:], func=AF.Square, accum_out=ss[:, 0:1]), reads=[xt], writes=[junk, ss])
                            kb.op('act', lambda e: e.activation(out=ss[:, 1:2], in_=ss[:, 0:1], func=AF.Ln, scale=1.0 / D, bias=epsc[:, 0:1]), reads=[ss, epsc], writes=[ss])
                            kb.op('act', lambda e: e.activation(out=ss[:, 2:3], in_=ss[:, 1:2], func=AF.Exp, scale=-0.5), reads=[ss], writes=[ss])
                        steps.append(s_stat)

                        def s_xs():
                            kb.op('dve', lambda e: e.tensor_scalar(out=xs[:], in0=xt[:], scalar1=ss[:, 2:3], scalar2=None, op0=ALU.mult), reads=[xt, ss], writes=[xs])
                        steps.append(s_xs)

                        def s_tr():
                            for hlf in range(2):
                                for kk in range(4):
                                    k = hlf * 4 + kk
                                    kb.op('pe', lambda e: e.transpose(psum[2 + hlf][:, kk * 128:(kk + 1) * 128], xs[:, k * 128:(k + 1) * 128], identF[:]),
                                          reads=[xs, identF], writes=[psum[2 + hlf]], chain=(kk > 0))
                        steps.append(s_tr)

                        def s_ev():
                            for hlf in range(2):
                                for kk in range(4):
                                    k = hlf * 4 + kk
                                    kb.op('dve', lambda e: e.tensor_scalar(out=h2T[:, k, :], in0=psum[2 + hlf][:, kk * 128:(kk + 1) * 128], scalar1=sc2[:, k:k + 1],
                                                                           scalar2=modT[:, 24 + k, 0:1], op0=ALU.mult, op1=ALU.add), reads=[psum[2 + hlf], modT, sc2], writes=[h2T])
                        steps.append(s_ev)

                        def mk_q(g):
                            def f():
                                wb = wbs[g]
                                for c in range(4):
                                    for k in range(8):
                                        kb.op('pe', lambda e: e.matmul(psum[2 + g % 2][:, c * 128:(c + 1) * 128], lhsT=wb[:, k, c * 128:(c + 1) * 128], rhs=h2T[:, k, :],
                                                                       start=(k == 0), stop=(k == 7)), reads=[wb, h2T], writes=[psum[2 + g % 2]], chain=(k > 0))
                                if g + 1 < 4:
                                    wbs[g + 1] = load_w(wq_d[:, (g + 1) * 512:(g + 2) * 512], 512)
                                if g > 0:
                                    kb.op('dve', lambda e: e.tensor_copy(out=qTp[:, 4 * (g - 1):4 * g, :].rearrange("p m t -> p (m t)"), in_=psum[2 + (g - 1) % 2][:]),
                                          reads=[psum[2 + (g - 1) % 2]], writes=[qTp])
                            return f
                        for g in range(4):
                            steps.append(mk_q(g))

                        def s_q3():
                            kb.op('dve', lambda e: e.tensor_copy(out=qTp[:, 12:16, :].rearrange("p m t -> p (m t)"), in_=psum[3][:]), reads=[psum[3]], writes=[qTp])
                        steps.append(s_q3)

                        def mk_sc(g):
                            def f():
                                pbk = psum[2 + g % 2]
                                if g < 4:
                                    for c in range(4):
                                        ph_ = 4 * g + c
                                        kb.op('pe', lambda e: e.matmul(pbk[:, c * 128:(c + 1) * 128], lhsT=qTp[:, ph_, :], rhs=keysT[:, ph_ * 128:(ph_ + 1) * 128], start=True, stop=True),
                                              reads=[qTp, keysT], writes=[pbk], chain=(c > 0))
                                if g > 0:
                                    pbp = psum[2 + (g - 1) % 2]
                                    kb.op('dve', lambda e: e.tensor_copy(out=sc[:, 4 * (g - 1):4 * g, :].rearrange("p m t -> p (m t)"), in_=pbp[:]), reads=[pbp], writes=[sc])
                            return f
                        for g in range(5):
                            steps.append(mk_sc(g))

                        def mk_top(g):
                            def f():
                                for gg in (g, g + 1):
                                    kb.op('dve', lambda e: e.max(out=tv[:, gg, 0:8], in_=sc[:, gg, :]), reads=[sc], writes=[tv])
                                    kb.op('dve', lambda e: e.match_replace(out=work[:, 0:128], in_to_replace=tv[:, gg, 0:8], in_values=sc[:, gg, :], imm_value=NEG),
                                          reads=[sc, tv], writes=[work])
                                    kb.op('dve', lambda e: e.max(out=tv[:, gg, 8:16], in_=work[:, 0:128]), reads=[work], writes=[tv])
                            return f
                        for g in range(0, 16, 2):
                            steps.append(mk_top(g))

                        def mk_cand(p0):
                            def f():
                                for p in (p0, p0 + 1):
                                    kb.op('dve', lambda e: e.tensor_tensor(out=cand[:], in0=tv[:, 2 * p, :].unsqueeze(2).broadcast_to([128, 16, 16]),
                                                                           in1=tv[:, 2 * p + 1, :].unsqueeze(1).broadcast_to([128, 16, 16]), op=ALU.add), reads=[tv], writes=[cand])
                                    cf = cand[:].rearrange("p a b -> p (a b)")
                                    kb.op('dve', lambda e: e.max(out=c16[:, p, 0:8], in_=cf), reads=[cand], writes=[c16])
                                    kb.op('dve', lambda e: e.match_replace(out=work[:], in_to_replace=c16[:, p, 0:8], in_values=cf, imm_value=NEG), reads=[cand, c16], writes=[work])
                                    kb.op('dve', lambda e: e.max(out=c16[:, p, 8:16], in_=work[:]), reads=[work], writes=[c16])
                            return f
                        for p0 in range(0, 8, 2):
                            steps.append(mk_cand(p0))

                        def s_negm():
                            kb.op('dve', lambda e: e.tensor_scalar(out=st8[:, 0, :], in0=c16[:, :, 0], scalar1=-1.0, scalar2=None, op0=ALU.mult), reads=[c16], writes=[st8])
                            kb.op('dve', lambda e: e.tensor_scalar(out=st8[:, 4, :], in0=c16[:, :, 15], scalar1=-1.0, scalar2=1.0e-6, op0=ALU.mult, op1=ALU.add), reads=[c16], writes=[st8])
                        steps.append(s_negm)

                        def s_z():
                            for p in range(8):
                                kb.op('act', lambda e: e.activation(out=e16[:], in_=c16[:, p, :], func=AF.Exp, bias=st8[:, 0, p:p + 1], accum_out=st8[:, 1, p:p + 1]),
                                      reads=[c16, st8], writes=[e16, st8])
                            kb.op('act', lambda e: e.activation(out=st8[:, 2, :], in_=st8[:, 1, :], func=AF.Ln), reads=[st8], writes=[st8])
                        steps.append(s_z)

                        def s_cb():
                            kb.op('dve', lambda e: e.tensor_tensor(out=st8[:, 3, :], in0=st8[:, 0, :], in1=st8[:, 2, :], op=ALU.subtract), reads=[st8], writes=[st8])
                            kb.op('dve', lambda e: e.tensor_tensor(out=st8[:, 3, :], in0=st8[:, 3, :], in1=st8[:, 4, :], op=ALU.subtract), reads=[st8], writes=[st8])
                        steps.append(s_cb)

                        for f in steps:
                            f()
                            yield

                    def stageB(i):
                        sc, c16, st8 = sc2_[i % 2], c162[i % 2], st82[i % 2]
                        its = [(part, p) for part in range(8) for p in range(8)]
                        nI = len(its)
                        for n in range(nI + 4):
                            if n < nI and its[n][1] == 0:
                                yield its[n][0]
                            m = n - 4
                            if 0 <= m < nI:
                                part, p = its[m]
                                eq = Eq[m % 3]
                                wsl = W[:, part * 2048:(part + 1) * 2048].rearrange("p (a b) -> p a b", b=128)
                                if p > 0:
                                    kb.op('dve', lambda e: e.tensor_tensor(out=wsl, in0=wsl, in1=eq[:], op=ALU.add), reads=[Wb[part], eq], writes=[Wb[part]])
                            m = n - 2
                            if 0 <= m < nI:
                                part, p = its[m]
                                sq, eq = Sq[m % 3], Eq[m % 3]
                                wsl = W[:, part * 2048:(part + 1) * 2048].rearrange("p (a b) -> p a b", b=128)
                                sqf = sq[:].rearrange("p a b -> p (a b)")
                                if m % 3 != 2:
                                    kb.op('act', lambda e: e.activation(out=sqf, in_=sqf, func=AF.Prelu, alpha=1.0e30), reads=[sq], writes=[sq])
                                if p == 0:
                                    kb.op('act', lambda e: e.activation(out=wsl, in_=sq[:], func=AF.Exp, bias=st8[:, 3, p:p + 1]), reads=[sq, st8], writes=[Wb[part]])
                                else:
                                    kb.op('act', lambda e: e.activation(out=eq[:], in_=sq[:], func=AF.Exp, bias=st8[:, 3, p:p + 1]), reads=[sq, st8], writes=[eq])
                            if n < nI:
                                part, p = its[n]
                                sq = Sq[n % 3]
                                kb.op('dve', lambda e: e.scalar_tensor_tensor(out=sq[:], in0=sc[:, 2 * p, part * 16:(part + 1) * 16].unsqueeze(2).broadcast_to([128, 16, 128]),
                                                                              scalar=st8[:, 4, p:p + 1], in1=sc[:, 2 * p + 1, :].unsqueeze(1).broadcast_to([128, 16, 128]),
                                                                              op0=ALU.add, op1=ALU.add), reads=[sc, st8], writes=[sq])
                                if n % 3 == 2:
                                    kb.op('dve', lambda e: e.scalar_tensor_tensor(out=sq[:], in0=sq[:], scalar=1.0e30, in1=sq[:], op0=ALU.mult, op1=ALU.min),
                                          reads=[sq], writes=[sq])
                            yield None

                    def pull(gen, limit_part=None, state=None):
                        if gen is None:
                            return False
                        if state is not None and state.get('pending') is not None:
                            if limit_part is not None and state['pending'] > limit_part:
                                return True
                            state['pending'] = None
                        try:
                            r = next(gen)
                        except StopIteration:
                            return False
                        if r is not None and state is not None:
                            if limit_part is not None and r > limit_part:
                                state['pending'] = r
                        return True

                    def stageC(i, genB, stB, genA, j_from=-3, nxt=None):
                        s3 = i % NB3
                        xt, h2T = xt3[s3], h2T3[s3]
                        hcur = [h2T]
                        tsl = slice(i * 128, (i + 1) * 128)
                        aliveB, aliveA = genB is not None, genA is not None

                        def uload(eg):
                            u_ = ub[eg % 2]
                            kb.dma('sp', u_[:], uTb_d[:, eg * 512:(eg + 1) * 512].rearrange("(k p) n -> p k n", p=128), reads=[uTb_b], writes=[u_])

                        def vload(eg):
                            v_ = vbf[eg % 2]
                            kb.dma('sp', v_[:], vb_d[eg * 512:(eg + 1) * 512, :].rearrange("(c p) n -> p c n", p=128), reads=[vb_b], writes=[v_])

                        def mmA(eg):
                            pa = psum[4 + eg % 2]
                            u_ = ub[eg % 2]
                            for k in range(8):
                                kb.op('pe', lambda e: e.matmul(pa[:], lhsT=hcur[0][:, k, :], rhs=u_[:, k, :], start=(k == 0), stop=(k == 7)), reads=[hcur[0], u_], writes=[pa], chain=(k > 0))

                        def gel(eg):
                            pa = psum[4 + eg % 2]
                            g_ = Gt[eg % 2]
                            kb.op('act', lambda e: e.activation(out=g_[:], in_=pa[:], func=AF.Gelu), reads=[pa], writes=[g_])
                            kb.op('pool', lambda e: e.tensor_tensor(out=Xt[eg % 2][:], in0=g_[:], in1=W[:, eg * 512:(eg + 1) * 512], op=ALU.mult),
                                  reads=[g_, Wb[eg // 4]], writes=[Xt[eg % 2]])

                        def trn(eg):
                            p6 = pbf(6 + eg % 2)
                            for c in range(4):
                                kb.op('pe', lambda e: e.transpose(p6[:, c * 128:(c + 1) * 128], Xt[eg % 2][:, c * 128:(c + 1) * 128], identB[:]),
                                      reads=[Xt[eg % 2], identB], writes=[psum[6 + eg % 2]], chain=(c > 0))

                        def cpy(eg):
                            p6 = pbf(6 + eg % 2)
                            kb.op('act', lambda e: e.copy(out=XT[eg % 2][:].rearrange("p c t -> p (c t)"), in_=p6[:, 0:512]), reads=[psum[6 + eg % 2]], writes=[XT[eg % 2]])

                        def mmV(eg):
                            v_ = vbf[eg % 2]
                            for hf in range(2):
                                for c in range(4):
                                    kb.op('pe', lambda e: e.matmul(psum[hf][:], lhsT=XT[eg % 2][:, c, :], rhs=v_[:, c, hf * 512:(hf + 1) * 512],
                                                                   start=(eg == 0 and c == 0), stop=(eg == NEG_ - 1 and c == 3)), reads=[XT[eg % 2], v_], writes=[psum[hf]], chain=(hf + c > 0))

                        def c_ops(j):
                            if 0 <= j - 1 < NEG_:
                                mmV(j - 1)
                            if 0 <= j + 1 < NEG_:
                                vload(j + 1)
                            if 0 <= j + 3 < NEG_:
                                uload(j + 3)
                            if 0 <= j + 2 < NEG_:
                                mmA(j + 2)
                            if 0 <= j + 1 < NEG_:
                                gel(j + 1)
                            if 0 <= j < NEG_:
                                trn(j)

                        for j in range(j_from, NEG_ + 1):
                            c_ops(j)
                            lim = (j + 2) // 4 - 1
                            for it_ in range(4):
                                if aliveB:
                                    aliveB = pull(genB, lim, stB)
                                    if stB.get('pending') is not None and aliveA:
                                        aliveA = pull(genA)
                                elif aliveA:
                                    aliveA = pull(genA)
                                if it_ == 0 and 0 <= j < NEG_:
                                    cpy(j)
                        if nxt is not None:
                            hcur[0] = nxt
                            for j in range(-3, 0):
                                c_ops(j)
                            hcur[0] = h2T
                        for hf in range(2):
                            kb.op('dve', lambda e: e.tensor_tensor(out=x2[:, hf * 512:(hf + 1) * 512], in0=psum[hf][:], in1=g2rep[:, hf * 512:(hf + 1) * 512], op=ALU.mult),
                                  reads=[psum[hf], g2rep], writes=[x2])
                        kb.op('pool', lambda e: e.tensor_tensor(out=x2[:], in0=x2[:], in1=xt[:], op=ALU.add), reads=[x2, xt], writes=[x2])
                        ss = ss2[1]
                        kb.op('act', lambda e: e.activation(out=junk[:], in_=x2[:], func=AF.Square, accum_out=ss[:, 0:1]), reads=[x2], writes=[junk, ss])
                        kb.op('act', lambda e: e.activation(out=ss[:, 1:2], in_=ss[:, 0:1], func=AF.Ln, scale=1.0 / D, bias=epsc[:, 0:1]), reads=[ss, epsc], writes=[ss])
                        kb.op('act', lambda e: e.activation(out=ss[:, 2:3], in_=ss[:, 1:2], func=AF.Exp, scale=-0.5), reads=[ss], writes=[ss])
                        kb.op('dve', lambda e: e.scalar_tensor_tensor(out=x2[:], in0=x2[:], scalar=ss[:, 2:3], in1=nfrep[:], op0=ALU.mult, op1=ALU.mult),
                              reads=[x2, ss, nfrep], writes=[x2])
                        kb.dma('sp', y_d[b, tsl, :], x2[:], reads=[x2], writes=[y_b])
                        while aliveB:
                            aliveB = pull(genB, 99, stB)
                        while aliveA:
                            aliveA = pull(genA)

                    for _ in stageA(0):
                        pass
                    if NL > 1:
                        for _ in stageA(1):
                            pass
                    for _ in stageB(0):
                        pass
                    for i in range(NL):
                        gB = stageB(i + 1) if i + 1 < NL else None
                        gA = stageA(i + 2) if i + 2 < NL else None
                        stageC(i, gB, {'pending': None}, gA, j_from=(-3 if i == 0 else 0), nxt=(h2T3[(i + 1) % NB3] if i + 1 < NL else None))
                kb.barrier([y_b])
        kb.wait_all('sp', [y_b] + [x1_b[b][jl] for b in range(NSEQ) for jl in range(NL)])
    return nc


_CACHE = {}


def _prep_shared(inp):
    f = np.float32
    w_in = inp["w_in"][0]
    mq, mk, mv, mo = w_in[:, 0:512], w_in[:, 512:1024], w_in[:, 1024:1536], w_in[:, 1536:2048]
    mg = w_in[:, 2048:2064]
    gq, gk, gv, gr = w_in[:, 2064:2320], w_in[:, 2320:2576], w_in[:, 2576:3088], w_in[:, 3088:3600]
    glr = w_in[:, 3600:3632]
    glrpad = np.zeros((D, 128), f)
    glrpad[:, 0:16] = glr[:, 0:16]
    glrpad[:, 32:48] = glr[:, 16:32]
    lrw2 = np.zeros((64, 256), f)
    lrw2[0:16] = inp["gla_lr_w2"][0, 0]
    lrw2[32:48] = inp["gla_lr_w2"][0, 1]
    sh = {
        "w_mod": np.ascontiguousarray(inp["w_mod"][0]),
        "bmodT": np.ascontiguousarray(inp["b_mod"][0].reshape(48, 128).T),
        "b_mod": np.ascontiguousarray(inp["b_mod"][0].reshape(1, -1)),
        "ngT": np.ascontiguousarray(np.stack([inp["norm1_g"][0].reshape(8, 128).T, inp["norm2_g"][0].reshape(8, 128).T], axis=1)),
        "w_fm": np.ascontiguousarray(np.concatenate([mq, mk, gq, gk, glrpad], axis=1)),
        "w_tm": np.ascontiguousarray(np.concatenate([mv, mo, gv, gr, mg], axis=1)),
        "cwT": np.ascontiguousarray(inp["ml_conv_w"][0].T.reshape(8, 128, 3).transpose(1, 0, 2)),
        "gate_b": np.ascontiguousarray(inp["ml_gate_b"][0].reshape(1, 16)),
        "ml_g": np.ascontiguousarray(inp["ml_norm_g"][0].reshape(1, 512)),
        "gla_g": np.ascontiguousarray(inp["gla_norm_g"][0].reshape(1, 512)),
        "lrw2": lrw2,
        "alpha_b": np.ascontiguousarray(inp["gla_alpha_b"][0].reshape(1, 512)),
        "w_out": np.ascontiguousarray(inp["w_out"][0]),
        "wq": np.ascontiguousarray(inp["peer_wq"][0]),
        "keysT": np.ascontiguousarray(inp["peer_keys"][0].transpose(3, 0, 1, 2).reshape(128, 2048)),
        "uT": np.ascontiguousarray(inp["peer_u"][0].T),
        "v": np.ascontiguousarray(inp["peer_v"][0]),
        "nf_g": np.ascontiguousarray(inp["norm_f_g"].reshape(1, D)),
    }
    return {k: np.asarray(v, dtype=f) for k, v in sh.items()}


def run(inp, n_cores, dbg=False):
    inp = {k: np.asarray(v) for k, v in inp.items()}
    B, N, _ = inp["x"].shape
    C = inp["ctx"].shape[1]
    NSEQ = B // n_cores
    key = (NSEQ, N, C, dbg)
    if key not in _CACHE:
        _CACHE[key] = build(NSEQ, N, C, dbg)
    nc = _CACHE[key]
    sh = _prep_shared(inp)
    in_maps = []
    for i in range(n_cores):
        sl = slice(i * NSEQ, (i + 1) * NSEQ)
        cc = np.concatenate([inp["c"][sl], inp["c_ctx"][None, :]], axis=0).astype(np.float32)
        m = dict(sh)
        m["x"] = np.ascontiguousarray(inp["x"][sl], dtype=np.float32)
        m["ctx"] = np.ascontiguousarray(inp["ctx"][sl], dtype=np.float32)
        m["ccT"] = np.ascontiguousarray(cc.reshape(NSEQ + 1, 8, 128).transpose(2, 1, 0))
        in_maps.append(m)
    res = run_bass_kernel_spmd(nc, in_maps, core_ids=list(range(n_cores)))
    y = np.concatenate([r["y"] for r in res.results], axis=0)
    if dbg:
        return y, np.concatenate([r["x1s"] for r in res.results], axis=0)
    return y


def kernel(**inputs):
    return run(inputs, 8).astype(np.float32)
```

```python
import math
from contextlib import ExitStack

import numpy as np
import concourse.bass as bass
import concourse.mybir as mybir
from concourse.bass_utils import run_bass_kernel_spmd

F32 = mybir.dt.float32
BF16 = mybir.dt.bfloat16
AF = mybir.ActivationFunctionType
ALU = mybir.AluOpType

D = 1024
EPS = 1e-6
NEG = -1.0e30
NEXP = 16384


class Buf:
    __slots__ = ("w", "r", "dsem", "dcnt")

    def __init__(s):
        s.w = None
        s.r = {}
        s.dsem = None
        s.dcnt = 0


class TB:
    def __init__(s, t):
        s.t = t
        s.b = Buf()

    def __getitem__(s, idx):
        return s.t[idx]


class KB:
    def __init__(s, nc, es):
        s.nc = nc
        s.es = es
        s.E = {'pe': nc.tensor, 'act': nc.scalar, 'dve': nc.vector, 'pool': nc.gpsimd, 'sp': nc.sync}
        s.sem = {}
        s.cnt = {}
        for k in s.E:
            s.sem[k] = es.enter_context(nc.semaphore("s_" + k))
            s.cnt[k] = 0
        s.waited = {k: {} for k in s.E}
        s.nsem = 0
        s.ninst = 0
        s.free = []
        s.swsems = set()
        s.dlast = {}

    def _deps(s, reads, writes):
        deps = {}
        for b in reads:
            b = b.b if isinstance(b, TB) else b
            if b.w is not None:
                k, v = b.w
                deps[k] = max(deps.get(k, 0), v)
        for b in writes:
            b = b.b if isinstance(b, TB) else b
            if b.w is not None:
                k, v = b.w
                deps[k] = max(deps.get(k, 0), v)
            for k, v in b.r.items():
                deps[k] = max(deps.get(k, 0), v)
        return deps

    def _need(s, eng, deps, skip_self=False):
        w = s.waited[eng]
        for k, v in deps.items():
            if skip_self and k == eng:
                continue
            if w.get(k, 0) < v:
                s.E[eng].wait_ge(s.sem[k], v)
                w[k] = v

    def _mark(s, me, reads, writes):
        k, v = me
        for b in reads:
            b = b.b if isinstance(b, TB) else b
            b.r[k] = max(b.r.get(k, 0), v)
        for b in writes:
            b = b.b if isinstance(b, TB) else b
            b.w = me
            b.r = {}

    def op(s, eng, fn, reads=(), writes=(), chain=False):
        s._need(eng, s._deps(reads, writes), skip_self=(chain and eng == 'pe'))
        ins = fn(s.E[eng])
        s.cnt[eng] += 1
        s.ninst += 1
        ins.then_inc(s.sem[eng], 1)
        s._mark((eng, s.cnt[eng]), reads, writes)
        return ins

    def dma(s, q, out_ap, in_ap, reads=(), writes=(), **kw):
        s._need(q, s._deps(reads, writes))
        tb = writes[0]
        tb = tb.b if isinstance(tb, TB) else tb
        if tb.dsem is None:
            if s.free and q != 'pool':
                tb.dsem, tb.dcnt = s.free.pop()
            else:
                if q == 'pool':
                    s.swsems.add("d%d" % s.nsem)
                key = "d%d" % s.nsem
                s.nsem += 1
                s.sem[key] = s.es.enter_context(s.nc.semaphore(key))
                tb.dsem = key
        ins = s.E[q].dma_start(out=out_ap, in_=in_ap, **kw)
        s.ninst += 1
        tb.dcnt += 16
        ins.then_inc(s.sem[tb.dsem], 16)
        s.dlast[tb.dsem] = tb.dcnt
        s._mark((tb.dsem, tb.dcnt), reads, writes)
        return ins

    def release(s, tb):
        b = tb.b if isinstance(tb, TB) else tb
        if b.dsem is not None:
            if b.dsem not in s.swsems:
                s.free.append((b.dsem, b.dcnt))
            b.dsem = None

    def wait_all(s, eng, bufs):
        s._need(eng, s._deps(bufs, ()))

    def barrier(s, dma_bufs=()):
        deps = {k: s.cnt[k] for k in s.E if s.cnt[k] > 0}
        deps.update(s.dlast)
        for e in s.E:
            s._need(e, dict(deps))


def build(NSEQ, N, C, dbg=False):
    NL = N // 128
    NCT = C // 128
    NT = NL + NCT
    T = N + C
    nc = bass.Bass("TRN2", target_bir_lowering=False)
    dt = lambda name, shape, dty=F32, kind="ExternalInput": nc.dram_tensor(name, shape, dty, kind=kind).ap()
    x_d = dt("x", [NSEQ, N, D])
    ctx_d = dt("ctx", [NSEQ, C, D])
    ccT_d = dt("ccT", [128, 8, NSEQ + 1])
    wmod_d = dt("w_mod", [D, 6 * D])
    bmodT_d = dt("bmodT", [128, 48])
    bmod_d = dt("b_mod", [1, 6 * D])
    ng_d = dt("ngT", [128, 2, 8])
    wfm_d = dt("w_fm", [D, 13 * 128])
    wtm_d = dt("w_tm", [D, 2064])
    cw_d = dt("cwT", [128, 8, 3])
    gb_d = dt("gate_b", [1, 16])
    mlg_d = dt("ml_g", [1, 512])
    glg_d = dt("gla_g", [1, 512])
    lrw_d = dt("lrw2", [64, 256])
    ab_d = dt("alpha_b", [1, 512])
    wout_d = dt("w_out", [D, D])
    wq_d = dt("wq", [D, 2048])
    keysT_d = dt("keysT", [128, 16 * 128])
    uT_d = dt("uT", [D, NEXP])
    v_d = dt("v", [NEXP, D])
    nf_d = dt("nf_g", [1, D])
    y_d = dt("y", [NSEQ, N, D], F32, "ExternalOutput")
    x1_d = dt("x1s", [NSEQ, N, D], F32, "ExternalOutput" if dbg else "Internal")
    uTb_d = dt("uTb", [D, NEXP], BF16, "Internal")
    vb_d = dt("vb", [NEXP, D], BF16, "Internal")

    with ExitStack() as es:
        kb = KB(nc, es)

        uid = [0]

        def sb(ctx, name, shape, dty=F32):
            uid[0] += 1
            tb = TB(ctx.enter_context(nc.sbuf_tensor("%s_t%d" % (name, uid[0]), shape, dty)))
            if ctx is not es:
                ctx.callback(kb.release, tb)
            return tb

        identF = sb(es, "identF", [128, 128])
        identB = sb(es, "identB", [128, 128], BF16)
        tri = [sb(es, "tri%d" % d, [128, 128]) for d in range(2)]
        ntri16 = [sb(es, "ntri%d" % d, [128, 128]) for d in range(2)]
        ptri16 = [sb(es, "ptri%d" % d, [128, 128]) for d in range(2)]
        ones1 = sb(es, "ones1", [1, 128])
        gbrep = sb(es, "gbrep", [128, 16])
        mlgrep = sb(es, "mlgrep", [128, 512])
        glgrep = sb(es, "glgrep", [128, 512])
        lrw = sb(es, "lrw", [64, 256])
        abrow = sb(es, "abrow", [1, 512])
        ngT = sb(es, "ngT", [128, 2, 8])
        cwT = sb(es, "cwT", [128, 8, 3])
        bmodT = sb(es, "bmodT", [128, 48])
        ccT = sb(es, "ccT", [128, 8, NSEQ + 1])
        epsc = sb(es, "epsc", [128, 1])
        onec = sb(es, "onec", [128, 1])
        lnks = sb(es, "lnks", [128, 1])
        lnqs = sb(es, "lnqs", [128, 1])
        lrwb = sb(es, "lrwb", [64, 256], BF16)
        ones1b = sb(es, "ones1b", [1, 128], BF16)
        abrowb = sb(es, "abrowb", [1, 512], BF16)
        psum = [TB(es.enter_context(nc.psum_tensor("pb%d" % i, [128, 512], F32))) for i in range(8)]

        def pbf(i):
            return psum[i].t[:].bitcast(BF16)

        kb.op('pool', lambda e: e.memset(identF[:], 0.0), writes=[identF])
        kb.op('pool', lambda e: e.affine_select(out=identF[:], in_=identF[:], pattern=[[-1, 128]], compare_op=ALU.not_equal,
                                                fill=1.0, base=0, channel_multiplier=1), reads=[identF], writes=[identF])
        kb.op('dve', lambda e: e.tensor_copy(out=identB[:], in_=identF[:]), reads=[identF], writes=[identB])
        for d in range(2):
            kb.op('pool', lambda e: e.memset(tri[d][:], 1.0), writes=[tri[d]])
            kb.op('pool', lambda e: e.affine_select(out=tri[d][:], in_=tri[d][:], pattern=[[1 if d == 0 else -1, 128]],
                                                    compare_op=ALU.is_ge, fill=0.0, base=0,
                                                    channel_multiplier=(-1 if d == 0 else 1)), reads=[tri[d]], writes=[tri[d]])
            kb.op('dve', lambda e: e.tensor_scalar(out=ntri16[d][:], in0=tri[d][:], scalar1=-1.0 / 16.0, scalar2=None, op0=ALU.mult),
                  reads=[tri[d]], writes=[ntri16[d]])
            kb.op('dve', lambda e: e.tensor_scalar(out=ptri16[d][:], in0=tri[d][:], scalar1=1.0 / 16.0, scalar2=None, op0=ALU.mult),
                  reads=[tri[d]], writes=[ptri16[d]])
        kb.op('pool', lambda e: e.memset(ones1[:], 1.0), writes=[ones1])
        kb.op('pool', lambda e: e.memset(ones1b[:], 1.0), writes=[ones1b])
        kb.op('pool', lambda e: e.memset(epsc[:], EPS), writes=[epsc])
        kb.op('pool', lambda e: e.memset(onec[:], 1.0), writes=[onec])
        kb.op('pool', lambda e: e.memset(lnks[:], -0.5 * math.log(128.0)), writes=[lnks])
        kb.op('pool', lambda e: e.memset(lnqs[:], math.log(0.125)), writes=[lnqs])
        kb.dma('sp', gbrep[:], gb_d.partition_broadcast(128), writes=[gbrep])
        kb.dma('sp', mlgrep[:], mlg_d.partition_broadcast(128), writes=[mlgrep])
        kb.dma('sp', glgrep[:], glg_d.partition_broadcast(128), writes=[glgrep])
        kb.dma('sp', lrw[:], lrw_d, writes=[lrw])
        kb.dma('sp', abrow[:], ab_d, writes=[abrow])
        kb.dma('sp', ngT[:], ng_d, writes=[ngT])
        kb.dma('sp', cwT[:], cw_d, writes=[cwT])
        kb.dma('sp', bmodT[:], bmodT_d, writes=[bmodT])
        kb.dma('sp', ccT[:], ccT_d, writes=[ccT])
        kb.op('dve', lambda e: e.tensor_copy(out=lrwb[:], in_=lrw[:]), reads=[lrw], writes=[lrwb])
        kb.op('dve', lambda e: e.tensor_copy(out=abrowb[:], in_=abrow[:]), reads=[abrow], writes=[abrowb])

        uTb_b = Buf()
        vb_b = Buf()
        x1_b = [[Buf()] * NL for _ in range(NSEQ)]
        y_b = Buf()

        with ExitStack() as ph:
            st = [sb(ph, "cst%d" % i, [128, 4096]) for i in range(2)]
            sb16 = [sb(ph, "csb%d" % i, [128, 4096], BF16) for i in range(2)]
            i = 0
            engs = ['act', 'dve']
            for (src, dst, dstb) in ((uT_d, uTb_d, uTb_b), (v_d, vb_d, vb_b)):
                rows, cols = src.shape
                sv = src.rearrange("(a p) c -> p a c", p=128)
                dv = dst.rearrange("(a p) c -> p a c", p=128)
                na = rows // 128
                cw = 4096 // 1
                for a in range(na):
                    for c0 in range(0, cols, 4096):
                        j = i % 2
                        w_ = min(4096, cols - c0)
                        kb.dma('sp', st[j][:, 0:w_], sv[:, a, c0:c0 + w_], writes=[st[j]])
                        eng = engs[i % 2]
                        if eng == 'act':
                            kb.op('act', lambda e: e.copy(out=sb16[j][:, 0:w_], in_=st[j][:, 0:w_]), reads=[st[j]], writes=[sb16[j]])
                        else:
                            kb.op(eng, lambda e: e.tensor_copy(out=sb16[j][:, 0:w_], in_=st[j][:, 0:w_]), reads=[st[j]], writes=[sb16[j]])
                        kb.dma('sp', dv[:, a, c0:c0 + w_], sb16[j][:, 0:w_], reads=[sb16[j]], writes=[dstb])
                        i += 1
        kb.barrier([uTb_b, vb_b])

        for b in range(NSEQ):
            with ExitStack() as sq:
                modT = sb(sq, "modT", [128, 48, 2])
                sc1 = sb(sq, "sc1", [128, 8, 2])
                sc2 = sb(sq, "sc2", [128, 8])
                g1rep = sb(sq, "grep", [128, D])
                g2rep = g1rep
                condT = sb(sq, "condT", [128, 8, 2], BF16)
                condrep = sb(sq, "condrep", [128, 8, 128], BF16)
                wbuf = [sb(sq, "wbuf%d" % i, [128, 8, 512], BF16) for i in range(2)]
                wcnt = [0]

                def load_w(src_ap, ncols):
                    j = wcnt[0] % 2
                    wcnt[0] += 1
                    kb.dma('pool', wbuf[j][:, :, 0:ncols], src_ap.rearrange("(k p) n -> p k n", p=128), writes=[wbuf[j]])
                    return wbuf[j]

                kb.op('act', lambda e: e.activation(out=condT[:, :, 0:1], in_=ccT[:, :, b:b + 1], func=AF.Silu), reads=[ccT], writes=[condT])
                kb.op('act', lambda e: e.activation(out=condT[:, :, 1:2], in_=ccT[:, :, NSEQ:NSEQ + 1], func=AF.Silu), reads=[ccT], writes=[condT])
                kb.op('dve', lambda e: e.tensor_copy(out=condrep[:], in_=condT[:, :, 0:1].broadcast_to([128, 8, 128])), reads=[condT], writes=[condrep])
                pm = psum[0]
                with ExitStack() as ph:
                    for g in range(12):
                        wb = load_w(wmod_d[:, g * 512:(g + 1) * 512], 512)
                        for c in range(4):
                            n = g * 4 + c
                            for k in range(8):
                                kb.op('pe', lambda e: e.matmul(pm[:, 2 * n:2 * n + 2], lhsT=wb[:, k, c * 128:(c + 1) * 128], rhs=condT[:, k, :],
                                                               start=(k == 0), stop=(k == 7)), reads=[wb, condT], writes=[pm], chain=(k > 0))
                    kb.op('dve', lambda e: e.tensor_tensor(out=modT[:], in0=pm[:, 0:96].rearrange("p (n r) -> p n r", r=2),
                                                           in1=bmodT[:].unsqueeze(2).broadcast_to([128, 48, 2]), op=ALU.add),
                          reads=[pm, bmodT], writes=[modT])
                    for r in range(2):
                        kb.op('dve', lambda e: e.scalar_tensor_tensor(out=sc1[:, :, r], in0=modT[:, 8:16, r], scalar=1.0, in1=ngT[:, 0, :],
                                                                      op0=ALU.add, op1=ALU.mult), reads=[modT, ngT], writes=[sc1])
                    kb.op('dve', lambda e: e.scalar_tensor_tensor(out=sc2[:], in0=modT[:, 32:40, 0], scalar=1.0, in1=ngT[:, 1, :],
                                                                  op0=ALU.add, op1=ALU.mult), reads=[modT, ngT], writes=[sc2])
                kb.barrier()

                def grep_build(ga, gb_):
                    with ExitStack() as ph:
                        brep = sb(ph, "brep", [128, 512])
                        for half, g in enumerate((ga, gb_)):
                            wb = load_w(wmod_d[:, g * 512:(g + 1) * 512], 512)
                            for k in range(8):
                                kb.op('pe', lambda e: e.matmul(psum[1][:], lhsT=condrep[:, k, :], rhs=wb[:, k, :], start=(k == 0), stop=(k == 7)),
                                      reads=[wb, condrep], writes=[psum[1]], chain=(k > 0))
                            kb.dma('sp', brep[:], bmod_d[:, g * 512:(g + 1) * 512].partition_broadcast(128), writes=[brep])
                            kb.op('dve', lambda e: e.tensor_tensor(out=g1rep[:, half * 512:(half + 1) * 512], in0=psum[1][:], in1=brep[:], op=ALU.add),
                                  reads=[psum[1], brep], writes=[g1rep])
                    kb.barrier()

                def norm_T(ph_tmp, src_ap, src_bufs, scale_col, bias_col, out_fn, out_buf, pidx=(0, 1)):
                    xt, junk, ss, xs = ph_tmp
                    kb.dma('sp', xt[:], src_ap, reads=src_bufs, writes=[xt])
                    kb.op('act', lambda e: e.activation(out=junk[:], in_=xt[:], func=AF.Square, accum_out=ss[:, 0:1]), reads=[xt], writes=[junk, ss])
                    kb.op('act', lambda e: e.activation(out=ss[:, 1:2], in_=ss[:, 0:1], func=AF.Ln, scale=1.0 / D, bias=epsc[:, 0:1]), reads=[ss, epsc], writes=[ss])
                    kb.op('act', lambda e: e.activation(out=ss[:, 2:3], in_=ss[:, 1:2], func=AF.Exp, scale=-0.5), reads=[ss], writes=[ss])
                    kb.op('dve', lambda e: e.tensor_scalar(out=xs[:], in0=xt[:], scalar1=ss[:, 2:3], scalar2=None, op0=ALU.mult), reads=[xt, ss], writes=[xs])
                    for hlf in range(2):
                        pb = psum[pidx[hlf]]
                        for kk in range(4):
                            k = hlf * 4 + kk
                            kb.op('pe', lambda e: e.transpose(pb[:, kk * 128:(kk + 1) * 128], xs[:, k * 128:(k + 1) * 128], identF[:]),
                                  reads=[xs, identF], writes=[pb])
                        for kk in range(4):
                            k = hlf * 4 + kk
                            eng = 'act' if kk % 2 == 0 else 'dve'
                            if eng == 'act':
                                kb.op('act', lambda e: e.activation(out=out_fn(k), in_=pb[:, kk * 128:(kk + 1) * 128], func=AF.Identity,
                                                                    scale=scale_col(k), bias=bias_col(k)), reads=[pb, modT, sc1, sc2], writes=[out_buf])
                            else:
                                kb.op('dve', lambda e: e.tensor_scalar(out=out_fn(k), in0=pb[:, kk * 128:(kk + 1) * 128], scalar1=scale_col(k),
                                                                       scalar2=bias_col(k), op0=ALU.mult, op1=ALU.add), reads=[pb, modT, sc1, sc2], writes=[out_buf])

                with ExitStack() as pm_:
                    hT = sb(pm_, "hT", [128, 8, T], BF16)
                    mixm = sb(pm_, "mixm", [128, NL, 512], BF16)
                    gates = sb(pm_, "gates", [128, NT, 16])
                    lfp = sb(pm_, "lfp", [128, NT, 8])
                    glrT = sb(pm_, "glrT", [128, T], BF16)
                    with ExitStack() as ph:
                        tmp = (sb(ph, "xt", [128, D]), sb(ph, "junk", [128, D]), sb(ph, "ss", [128, 4]), sb(ph, "xs", [128, D]))
                        for j in range(NT):
                            isctx = j < NCT
                            r = 1 if isctx else 0
                            src = ctx_d[b, j * 128:(j + 1) * 128, :] if isctx else x_d[b, (j - NCT) * 128:(j - NCT + 1) * 128, :]
                            norm_T(tmp, src, [], lambda k: sc1[:, k, r:r + 1], lambda k: modT[:, k, r:r + 1],
                                   lambda k: hT[:, k, j * 128:(j + 1) * 128], hT)
                    kb.barrier()

                    groups = [(g0, min(512, T - g0)) for g0 in range(0, T, 512)]

                    def proj_fm(ph, wb, coff, dst_fn, dst_buf, conv_k=None, zraw=None, zc=None):
                        for gi, (g0, gl) in enumerate(groups):
                            pb = psum[2 + gi % 2]
                            for k in range(8):
                                kb.op('pe', lambda e: e.matmul(pb[:, 0:gl], lhsT=wb[:, k, coff:coff + 128], rhs=hT[:, k, g0:g0 + gl],
                                                               start=(k == 0), stop=(k == 7)), reads=[wb, hT], writes=[pb], chain=(k > 0))
                            if conv_k is None:
                                kb.op('act', lambda e: e.copy(out=dst_fn(g0, gl), in_=pb[:, 0:gl]), reads=[pb], writes=[dst_buf])
                            else:
                                kb.op('act', lambda e: e.copy(out=zraw[:, g0:g0 + gl], in_=pb[:, 0:gl]), reads=[pb], writes=[zraw])
                        if conv_k is not None:
                            w0 = cwT[:, conv_k, 0:1]
                            w1 = cwT[:, conv_k, 1:2]
                            w2 = cwT[:, conv_k, 2:3]
                            kb.op('dve', lambda e: e.tensor_scalar(out=zc[:], in0=zraw[:], scalar1=w1, scalar2=None, op0=ALU.mult), reads=[zraw, cwT], writes=[zc])
                            regions = [(0, 1, C), (C, N // 64, 64)]
                            for (r0, nr, rl) in regions:
                                zr = zraw[:, r0:r0 + nr * rl].rearrange("p (a b) -> p a b", b=rl)
                                yr = zc[:, r0:r0 + nr * rl].rearrange("p (a b) -> p a b", b=rl)
                                kb.op('dve', lambda e: e.scalar_tensor_tensor(out=yr[:, :, 1:rl], in0=zr[:, :, 0:rl - 1], scalar=w0, in1=yr[:, :, 1:rl],
                                                                              op0=ALU.mult, op1=ALU.add), reads=[zraw, cwT, zc], writes=[zc])
                                kb.op('dve', lambda e: e.scalar_tensor_tensor(out=yr[:, :, 0:rl - 1], in0=zr[:, :, 1:rl], scalar=w2, in1=yr[:, :, 0:rl - 1],
                                                                              op0=ALU.mult, op1=ALU.add), reads=[zraw, cwT, zc], writes=[zc])
                            kb.op('act', lambda e: e.activation(out=dst_fn(0, T), in_=zc[:], func=AF.Silu), reads=[zc], writes=[dst_buf])

                    def proj_tm(wb, ncols, j, evac):
                        pb = psum[4 + j % 2]
                        for k in range(8):
                            kb.op('pe', lambda e: e.matmul(pb[:, 0:ncols], lhsT=hT[:, k, j * 128:(j + 1) * 128], rhs=wb[:, k, 0:ncols],
                                                           start=(k == 0), stop=(k == 7)), reads=[wb, hT], writes=[pb], chain=(k > 0))
                        evac(pb)

                    with ExitStack() as ph:
                        wb = load_w(wfm_d[:, 12 * 128:13 * 128], 128)
                        proj_fm(ph, wb, 0, lambda g0, gl: glrT[:, g0:g0 + gl], glrT)
                    kb.barrier()

                    def scan_pass(kind):
                        with ExitStack() as ar:
                            nq = 4 if kind == 0 else 2
                            qT = sb(ar, "qT", [128, nq, T], BF16)
                            kT = sb(ar, "kT", [128, nq, T], BF16)
                            vw = 129 if kind == 0 else 128
                            vt = sb(ar, "vt", [128, NT, 4, vw], BF16)
                            og = sb(ar, "og", [128, NL, 512], BF16)
                            with ExitStack() as ph:
                                zraw = sb(ph, "zraw", [128, T])
                                zc = sb(ph, "zc", [128, T])
                                if kind == 0:
                                    for half, dst in ((0, qT), (1, kT)):
                                        wb = load_w(wfm_d[:, half * 512:(half + 1) * 512], 512)
                                        for c in range(4):
                                            proj_fm(ph, wb, c * 128, (lambda g0, gl, c=c, dst=dst: dst[:, c, g0:g0 + gl]), dst,
                                                    conv_k=half * 4 + c, zraw=zraw, zc=zc)
                                    kb.op('pool', lambda e: e.memset(vt[:, :, :, 128:129], 1.0), writes=[vt])
                                    voff, ooff, ofn = 0, 512, AF.Sigmoid
                                else:
                                    wb = load_w(wfm_d[:, 1024:1536], 512)
                                    for c in range(4):
                                        dst = qT if c < 2 else kT
                                        proj_fm(ph, wb, c * 128, (lambda g0, gl, c=c, dst=dst: dst[:, c % 2, g0:g0 + gl]), dst)
                                    voff, ooff, ofn = 1024, 1536, AF.Silu
                                wb = load_w(wtm_d[:, voff:voff + 512], 512)
                                for j in range(NT):
                                    proj_tm(wb, 512, j, lambda pb: kb.op('act', lambda e: e.copy(out=vt[:, j, :, 0:128], in_=pb[:].rearrange("p (h e) -> p h e", e=128)),
                                                                           reads=[pb], writes=[vt]))
                                wb = load_w(wtm_d[:, ooff:ooff + 512], 512)
                                for j in range(NCT, NT):
                                    proj_tm(wb, 512, j, lambda pb: kb.op('act', lambda e: e.activation(out=og[:, j - NCT, :], in_=pb[:], func=ofn),
                                                                           reads=[pb], writes=[og]))
                                if kind == 0:
                                    wb = load_w(wtm_d[:, 2048:2064], 16)
                                    for j in range(NT):
                                        proj_tm(wb, 16, j, lambda pb: kb.op('dve', lambda e: e.tensor_tensor(out=gates[:, j, :], in0=pb[:, 0:16], in1=gbrep[:], op=ALU.add),
                                                                              reads=[pb, gbrep], writes=[gates]))
                                    gv4 = gates[:].rearrange("p j (d i h) -> p j d i h", d=2, i=2)
                                    for d in range(2):
                                        kb.op('act', lambda e: e.activation(out=lfp[:, :, d * 4:(d + 1) * 4], in_=gv4[:, :, d, 1, :], func=AF.Exp, scale=-1.0),
                                              reads=[gates], writes=[lfp])
                                    kb.op('act', lambda e: e.activation(out=lfp[:], in_=lfp[:], func=AF.Ln, bias=onec[:, 0:1]), reads=[lfp, onec], writes=[lfp])
                            kb.barrier()
                            with ExitStack() as ph:
                                S = sb(ph, "S", [128, 4, vw]) if kind == 0 else sb(ph, "S", [128, 2, 128])
                                Hb = sb(ph, "Hb", [128, NL, 512], BF16)
                                Sd = sb(ph, "Sd", S.t.shape)
                                Sbf = sb(ph, "Sbf", S.t.shape, BF16)
                                rep = [sb(ph, "rep%d" % i, [128, 4, 128]) for i in range(3)]
                                EB = sb(ph, "EB", [128, 4, 128])
                                EG = sb(ph, "EG", [128, 4, 128])
                                qt = sb(ph, "qt", [128, 4, 128], BF16)
                                kt = sb(ph, "kt", [128, 4, 128], BF16)
                                ktok = sb(ph, "ktok", [128, 4, 128], BF16)
                                PT = sb(ph, "PT", [128, 4, 128], BF16)
                                la = sb(ph, "la", [128, 256])
                                dec = sb(ph, "dec", [128, 4])
                                den = sb(ph, "den", [128, 8])
                                hn = sb(ph, "hn", [128, 4, 128])
                                ht = sb(ph, "ht", [128, 4, 128])
                                junk2 = sb(ph, "junk2", [128, 128])
                                hss = sb(ph, "hss", [128, 12])
                                if kind == 1:
                                    mix = sb(ph, "mix", [128, D], BF16)
                                    mixT = sb(ph, "mixT", [128, 8, 128], BF16)
                                    xres = sb(ph, "xres", [128, D])
                                    ytmp = sb(ph, "ytmp", [128, D])
                                    grep_build(4, 5)
                                    for hf_ in range(2):
                                        kb.dma('pool', wbuf[hf_][:], wout_d[:, hf_ * 512:(hf_ + 1) * 512].rearrange("(k p) n -> p k n", p=128), writes=[wbuf[hf_]])
                                grep_ = mlgrep if kind == 0 else glgrep
                                nh = 4
                                for d in (1, 0):
                                    order = list(range(NT)) if d == 0 else (list(range(NCT - 1, -1, -1)) + list(range(NT - 1, NCT - 1, -1)))
                                    last = 127 if d == 0 else 0
                                    kb.op('pool', lambda e: e.memset(S[:], 0.0), writes=[S])
                                    kb.op('pool', lambda e: e.memset(Sbf[:], 0.0), writes=[Sbf])
                                    for j in order:
                                        lat = j >= NCT
                                        jl = j - NCT
                                        tok = slice(j * 128, (j + 1) * 128)
                                        if kind == 0:
                                            lsrc = lfp[:, j, d * 4:(d + 1) * 4].unsqueeze(2).broadcast_to([128, 4, 128])
                                            isrc = gates[:, j, d * 8:d * 8 + 4].unsqueeze(2).broadcast_to([128, 4, 128])
                                            kb.op('dve', lambda e: e.tensor_scalar(out=rep[0][:], in0=lsrc, scalar1=-1.0, scalar2=None, op0=ALU.mult), reads=[lfp], writes=[rep[0]])
                                            kb.op('pool', lambda e: e.tensor_copy(out=rep[1][:], in_=lsrc), reads=[lfp], writes=[rep[1]])
                                            kb.op('pool', lambda e: e.tensor_copy(out=rep[2][:], in_=isrc), reads=[gates], writes=[rep[2]])
                                            for h in range(4):
                                                kb.op('pe', lambda e: e.matmul(psum[0][:, h * 128:(h + 1) * 128], lhsT=rep[0][:, h, :], rhs=tri[d][:], start=True, stop=True),
                                                      reads=[rep[0], tri[d]], writes=[psum[0]])
                                                kb.op('pe', lambda e: e.matmul(psum[1][:, h * 128:(h + 1) * 128], lhsT=rep[2][:, h, :], rhs=identF[:], start=True, stop=False),
                                                      reads=[rep[2], identF], writes=[psum[1]])
                                                kb.op('pe', lambda e: e.matmul(psum[1][:, h * 128:(h + 1) * 128], lhsT=rep[1][:, h, :], rhs=tri[d][:], start=False, stop=True),
                                                      reads=[rep[1], tri[d]], writes=[psum[1]])
                                            kb.op('act', lambda e: e.activation(out=EB[:].rearrange("p h t -> p (h t)"), in_=psum[0][:], func=AF.Exp), reads=[psum[0]], writes=[EB])
                                            kb.op('act', lambda e: e.activation(out=EG[:].rearrange("p h t -> p (h t)"), in_=psum[1][:], func=AF.Exp, bias=lnks[:, 0:1]),
                                                  reads=[psum[1], lnks], writes=[EG])
                                            kb.op('act', lambda e: e.copy(out=dec[:], in_=EB[:, :, last]), reads=[EB], writes=[dec])
                                            nb = 4
                                        else:
                                            kb.op('pe', lambda e: e.matmul(psum[0][:, 0:256], lhsT=glrT[32 * d:32 * d + 16, tok], rhs=lrwb[32 * d:32 * d + 16, :], start=True, stop=False),
                                                  reads=[glrT, lrwb], writes=[psum[0]])
                                            kb.op('pe', lambda e: e.matmul(psum[0][:, 0:256], lhsT=ones1b[:, :], rhs=abrowb[:, d * 256:(d + 1) * 256], start=False, stop=True),
                                                  reads=[ones1b, abrowb], writes=[psum[0]])
                                            kb.op('act', lambda e: e.activation(out=la[:], in_=psum[0][:, 0:256], func=AF.Exp, scale=-1.0), reads=[psum[0]], writes=[la])
                                            kb.op('act', lambda e: e.activation(out=la[:], in_=la[:], func=AF.Ln, bias=onec[:, 0:1]), reads=[la, onec], writes=[la])
                                            for c in range(2):
                                                kb.op('pe', lambda e: e.matmul(psum[1][:, c * 128:(c + 1) * 128], lhsT=la[:, c * 128:(c + 1) * 128], rhs=ntri16[d][:], start=True, stop=True),
                                                      reads=[la, ntri16[d]], writes=[psum[1]])
                                                kb.op('pe', lambda e: e.matmul(psum[1][:, 256 + c * 128:256 + (c + 1) * 128], lhsT=la[:, c * 128:(c + 1) * 128], rhs=ptri16[d][:], start=True, stop=True),
                                                      reads=[la, ptri16[d]], writes=[psum[1]])
                                            kb.op('act', lambda e: e.activation(out=EB[:, 0:2, :].rearrange("p h t -> p (h t)"), in_=psum[1][:, 0:256], func=AF.Exp, bias=lnqs[:, 0:1]),
                                                  reads=[psum[1], lnqs], writes=[EB])
                                            kb.op('act', lambda e: e.activation(out=EG[:, 0:2, :].rearrange("p h t -> p (h t)"), in_=psum[1][:, 256:512], func=AF.Exp), reads=[psum[1]], writes=[EG])
                                            kb.op('act', lambda e: e.activation(out=dec[:, 0:2], in_=psum[1][:, 0:256].rearrange("p (c t) -> p c t", t=128)[:, :, last], func=AF.Exp),
                                                  reads=[psum[1]], writes=[dec])
                                            nb = 2
                                        if lat:
                                            kb.op('dve', lambda e: e.tensor_tensor(out=qt[:, 0:nb, :], in0=qT[:, :, tok], in1=EB[:, 0:nb, :], op=ALU.mult), reads=[qT, EB], writes=[qt])
                                        kb.op('pool', lambda e: e.tensor_tensor(out=kt[:, 0:nb, :], in0=kT[:, :, tok], in1=EG[:, 0:nb, :], op=ALU.mult), reads=[kT, EG], writes=[kt])
                                        p2 = pbf(2)
                                        for c in range(nb):
                                            kb.op('pe', lambda e: e.transpose(p2[:, c * 128:(c + 1) * 128], kt[:, c, :], identB[:]), reads=[kt, identB], writes=[psum[2]])
                                        kb.op('act', lambda e: e.copy(out=ktok[:, 0:nb, :].rearrange("p h t -> p (h t)"), in_=p2[:, 0:nb * 128]), reads=[psum[2]], writes=[ktok])
                                        if lat:
                                            for h in range(4):
                                                if kind == 0:
                                                    l_, r_ = kt[:, h, :], qt[:, h, :]
                                                else:
                                                    c, hf = h // 2, h % 2
                                                    l_, r_ = kt[64 * hf:64 * hf + 64, c, :], qt[64 * hf:64 * hf + 64, c, :]
                                                if kind == 0:
                                                    sbk, scol = psum[3], h * 128
                                                else:
                                                    sbk, scol = (psum[3], psum[7])[h % 2], (h // 2) * 128
                                                kb.op('pe', lambda e: e.matmul(sbk[:, scol:scol + 128], lhsT=l_, rhs=r_, start=True, stop=True), reads=[kt, qt], writes=[sbk])
                                            if kind == 0:
                                                kb.op('dve', lambda e: e.tensor_tensor(out=PT[:], in0=psum[3][:].rearrange("p (h t) -> p h t", t=128),
                                                                                       in1=tri[d][:].unsqueeze(1).broadcast_to([128, 4, 128]), op=ALU.mult),
                                                      reads=[psum[3], tri[d]], writes=[PT])
                                            else:
                                                for hf in range(2):
                                                    sbk = (psum[3], psum[7])[hf]
                                                    kb.op('dve', lambda e: e.tensor_tensor(out=PT[:].rearrange("p (c f) t -> p c f t", f=2)[:, :, hf, :],
                                                                                           in0=sbk[:, 0:256].rearrange("p (c t) -> p c t", t=128),
                                                                                           in1=tri[d][:].unsqueeze(1).broadcast_to([128, 2, 128]), op=ALU.mult),
                                                          reads=[sbk, tri[d]], writes=[PT])
                                            for h in range(4):
                                                if kind == 0:
                                                    ob = psum[4 + h // 2]
                                                    oap = ob[:, (h % 2) * 129:(h % 2) * 129 + 129]
                                                    ql, sr = qt[:, h, :], Sbf[:, h, :]
                                                else:
                                                    c, hf = h // 2, h % 2
                                                    ob = psum[4 + hf]
                                                    oap = ob[:, c * 128:(c + 1) * 128]
                                                    ql, sr = qt[64 * hf:64 * hf + 64, c, :], Sbf[64 * hf:64 * hf + 64, c, :]
                                                kb.op('pe', lambda e: e.matmul(oap, lhsT=PT[:, h, :], rhs=vt[:, j, h, :], start=True, stop=False), reads=[PT, vt], writes=[ob])
                                                kb.op('pe', lambda e: e.matmul(oap, lhsT=ql, rhs=sr, start=False, stop=True), reads=[qt, Sbf], writes=[ob])
                                            if kind == 0:
                                                for bb in range(2):
                                                    o3 = psum[4 + bb][:, 0:258].rearrange("p (h e) -> p h e", e=129)
                                                    kb.op('act', lambda e: e.activation(out=den[:, bb * 2:bb * 2 + 2], in_=o3[:, :, 128], func=AF.Abs),
                                                          reads=[psum[4 + bb]], writes=[den])
                                                kb.op('dve', lambda e: e.tensor_scalar(out=den[:, 0:4], in0=den[:, 0:4], scalar1=1.0, scalar2=None, op0=ALU.max), reads=[den], writes=[den])
                                                kb.op('dve', lambda e: e.reciprocal(out=den[:, 4:8], in_=den[:, 0:4]), reads=[den], writes=[den])
                                                for bb in range(2):
                                                    o3 = psum[4 + bb][:, 0:258].rearrange("p (h e) -> p h e", e=129)
                                                    kb.op('dve', lambda e: e.tensor_tensor(out=hn[:, bb * 2:bb * 2 + 2, :], in0=o3[:, :, 0:128],
                                                                                           in1=den[:, 4 + bb * 2:6 + bb * 2].unsqueeze(2).broadcast_to([128, 2, 128]), op=ALU.mult),
                                                          reads=[psum[4 + bb], den], writes=[hn])
                                                hsrc = hn[:].rearrange("p h e -> p (h e)")
                                                hsrc_b = [hn]
                                            else:
                                                for hf in range(2):
                                                    kb.op('act', lambda e: e.copy(out=hn[:].rearrange("p (c f) e -> p c f e", f=2)[:, :, hf, :],
                                                                                  in_=psum[4 + hf][:, 0:256].rearrange("p (c e) -> p c e", e=128)),
                                                          reads=[psum[4 + hf]], writes=[hn])
                                                hsrc = hn[:].rearrange("p h e -> p (h e)")
                                                hsrc_b = [hn]
                                            if d == 1:
                                                kb.op('act', lambda e: e.copy(out=Hb[:, jl, :], in_=hsrc), reads=hsrc_b, writes=[Hb])
                                            else:
                                                kb.op('dve', lambda e: e.tensor_tensor(out=ht[:].rearrange("p h e -> p (h e)"), in0=hsrc, in1=Hb[:, jl, :], op=ALU.add),
                                                      reads=hsrc_b + [Hb], writes=[ht])
                                                for h in range(4):
                                                    kb.op('act', lambda e: e.activation(out=junk2[:], in_=ht[:, h, :], func=AF.Square, accum_out=hss[:, h:h + 1]),
                                                          reads=[ht], writes=[junk2, hss])
                                                kb.op('act', lambda e: e.activation(out=hss[:, 4:8], in_=hss[:, 0:4], func=AF.Ln, scale=1.0 / 128.0, bias=epsc[:, 0:1]),
                                                      reads=[hss, epsc], writes=[hss])
                                                kb.op('act', lambda e: e.activation(out=hss[:, 8:12], in_=hss[:, 4:8], func=AF.Exp, scale=-0.5), reads=[hss], writes=[hss])
                                                kb.op('dve', lambda e: e.tensor_tensor(out=ht[:], in0=ht[:], in1=hss[:, 8:12].unsqueeze(2).broadcast_to([128, 4, 128]), op=ALU.mult),
                                                      reads=[ht, hss], writes=[ht])
                                                kb.op('pool', lambda e: e.tensor_tensor(out=ht[:].rearrange("p h e -> p (h e)"), in0=ht[:].rearrange("p h e -> p (h e)"), in1=grep_[:], op=ALU.mult),
                                                      reads=[ht, grep_], writes=[ht])
                                                if kind == 0:
                                                    kb.op('dve', lambda e: e.tensor_tensor(out=mixm[:, jl, :], in0=ht[:].rearrange("p h e -> p (h e)"), in1=og[:, jl, :], op=ALU.mult),
                                                          reads=[ht, og], writes=[mixm])
                                                else:
                                                    kb.op('dve', lambda e: e.tensor_tensor(out=mix[:, 512:1024], in0=ht[:].rearrange("p h e -> p (h e)"), in1=og[:, jl, :], op=ALU.mult),
                                                          reads=[ht, og], writes=[mix])
                                                    kb.op('pool', lambda e: e.tensor_copy(out=mix[:, 0:512], in_=mixm[:, jl, :]), reads=[mixm], writes=[mix])
                                                    p6 = pbf(6)
                                                    for k in range(8):
                                                        kb.op('pe', lambda e: e.transpose(p6[:, k * 128:(k + 1) * 128], mix[:, k * 128:(k + 1) * 128], identB[:]),
                                                              reads=[mix, identB], writes=[psum[6]])
                                                    kb.op('act', lambda e: e.copy(out=mixT[:].rearrange("p k t -> p (k t)"), in_=p6[:, 0:1024]), reads=[psum[6]], writes=[mixT])
                                                    kb.dma('sp', xres[:], x_d[b, jl * 128:(jl + 1) * 128, :], writes=[xres])
                                                    for hf in range(2):
                                                        for k in range(8):
                                                            kb.op('pe', lambda e: e.matmul(psum[7][:], lhsT=mixT[:, k, :], rhs=wbuf[hf][:, k, :], start=(k == 0), stop=(k == 7)),
                                                                  reads=[mixT, wbuf[hf]], writes=[psum[7]], chain=(k > 0))
                                                        kb.op('dve', lambda e: e.tensor_tensor(out=ytmp[:, hf * 512:(hf + 1) * 512], in0=psum[7][:], in1=g1rep[:, hf * 512:(hf + 1) * 512], op=ALU.mult),
                                                              reads=[psum[7], g1rep], writes=[ytmp])
                                                    kb.op('pool', lambda e: e.tensor_tensor(out=ytmp[:], in0=ytmp[:], in1=xres[:], op=ALU.add), reads=[ytmp, xres], writes=[ytmp])
                                                    kb.dma('sp', x1_d[b, jl * 128:(jl + 1) * 128, :], ytmp[:], reads=[ytmp], writes=[x1_b[b][jl]])
                                        if kind == 0:
                                            for h in range(4):
                                                ub = psum[(0, 1)[h // 2]] if False else psum[6 + h // 2] if kind == 0 else None
                                                kb.op('pe', lambda e: e.matmul(ub[:, (h % 2) * 129:(h % 2) * 129 + 129], lhsT=ktok[:, h, :], rhs=vt[:, j, h, :], start=True, stop=True),
                                                      reads=[ktok, vt], writes=[ub])
                                            for bb in range(2):
                                                u3 = psum[6 + bb][:, 0:258].rearrange("p (h e) -> p h e", e=129)
                                                kb.op('dve', lambda e: e.tensor_tensor(out=Sd[:, bb * 2:bb * 2 + 2, :], in0=u3, in1=S[:, bb * 2:bb * 2 + 2, :], op=ALU.add),
                                                      reads=[psum[6 + bb], S], writes=[Sd])
                                            kb.op('pool', lambda e: e.tensor_tensor(out=S[:], in0=Sd[:], in1=dec[:].unsqueeze(2).broadcast_to([128, 4, vw]), op=ALU.mult),
                                                  reads=[Sd, dec], writes=[S])
                                        else:
                                            for h in range(4):
                                                c, hf = h // 2, h % 2
                                                kb.op('pe', lambda e: e.matmul(psum[5][64 * hf:64 * hf + 64, c * 128:(c + 1) * 128], lhsT=ktok[:, c, 64 * hf:64 * hf + 64], rhs=vt[:, j, h, :],
                                                                               start=True, stop=True), reads=[ktok, vt], writes=[psum[5]])
                                            kb.op('dve', lambda e: e.tensor_tensor(out=Sd[:], in0=psum[5][:, 0:256].rearrange("p (c e) -> p c e", e=128), in1=S[:], op=ALU.add),
                                                  reads=[psum[5], S], writes=[Sd])
                                            kb.op('pool', lambda e: e.tensor_tensor(out=S[:], in0=Sd[:], in1=dec[:, 0:2].unsqueeze(2).broadcast_to([128, 2, 128]), op=ALU.mult),
                                                  reads=[Sd, dec], writes=[S])
                                        kb.op('act', lambda e: e.copy(out=Sbf[:], in_=S[:]), reads=[S], writes=[Sbf])
                            kb.barrier()

                    scan_pass(0)
                    scan_pass(1)
                kb.barrier([x1_b[b][jl] for jl in range(NL)])

                with ExitStack() as pp:
                    NB3 = 3
                    xt3 = [sb(pp, "xt", [128, D]) for _ in range(NB3)]
                    h2T3 = [sb(pp, "h2T", [128, 8, 128], BF16) for _ in range(NB3)]
                    junk = sb(pp, "junk", [128, D], BF16)
                    xs = sb(pp, "xs", [128, D])
                    ss2 = [sb(pp, "ss", [128, 4]) for _ in range(2)]
                    nfrep = sb(pp, "nfrep", [128, D])
                    keysT = sb(pp, "keysT", [128, 16 * 128], BF16)
                    kb.dma('sp', nfrep[:], nf_d.partition_broadcast(128), writes=[nfrep])
                    kb.dma('pool', keysT[:], keysT_d, writes=[keysT])
                    grep_build(10, 11)
                    qTp = sb(pp, "qTp", [128, 16, 128], BF16)
                    sc2_ = [sb(pp, "sc", [128, 16, 128]) for _ in range(2)]
                    s2m = [sb(pp, "s2m", [128, 8, 128]) for _ in range(2)]
                    work = sb(pp, "work", [128, 256])
                    tv = sb(pp, "tv", [128, 16, 16])
                    cand = sb(pp, "cand", [128, 16, 16])
                    c162 = [sb(pp, "c16", [128, 8, 16]) for _ in range(2)]
                    e16 = sb(pp, "e16", [128, 16])
                    st82 = [sb(pp, "st8", [128, 5, 8]) for _ in range(2)]
                    W = sb(pp, "W", [128, NEXP], BF16)
                    Wb = [Buf() for _ in range(8)]
                    Sq = [sb(pp, "Sq", [128, 16, 128]) for _ in range(3)]
                    Eq = [sb(pp, "Eq", [128, 16, 128], BF16) for _ in range(3)]
                    ub = [sb(pp, "ub", [128, 8, 512], BF16) for _ in range(2)]
                    vbf = [sb(pp, "vbf", [128, 4, D], BF16) for _ in range(2)]
                    Gt = [sb(pp, "Gt", [128, 512], BF16) for _ in range(2)]
                    Xt = [sb(pp, "Xt", [128, 512], BF16) for _ in range(2)]
                    XT = [sb(pp, "XT", [128, 4, 128], BF16) for _ in range(2)]
                    x2 = xs
                    NEG_ = NEXP // 512

                    def stageA(i):
                        s3, s2_ = i % NB3, i % 2
                        xt, h2T, sc, c16, st8 = xt3[s3], h2T3[s3], sc2_[s2_], c162[s2_], st82[s2_]
                        tsl = slice(i * 128, (i + 1) * 128)
                        ss = ss2[0]
                        wbs = {}
                        steps = []

                        def s_load():
                            kb.dma('pool', xt[:], x1_d[b, tsl, :], reads=[x1_b[b][i]], writes=[xt])
                            wbs[0] = load_w(wq_d[:, 0:512], 512)
                        steps.append(s_load)
                        steps.append(lambda: None)

                        def s_stat():
                            kb.op('act', lambda e: e.activation(out=junk[:], in_=xt[:], func=AF.Square, accum_out=ss[:, 0:1]), reads=[xt], writes=[junk, ss])
                            kb.op('act', lambda e: e.activation(out=ss[:, 1:2], in_=ss[:, 0:1], func=AF.Ln, scale=1.0 / D, bias=epsc[:, 0:1]), reads=[ss, epsc], writes=[ss])
                            kb.op('act', lambda e: e.activation(out=ss[:, 2:3], in_=ss[:, 1:2], func=AF.Exp, scale=-0.5), reads=[ss], writes=[ss])
                        steps.append(s_stat)

                        def s_xs():
                            kb.op('dve', lambda e: e.tensor_scalar(out=xs[:], in0=xt[:], scalar1=ss[:, 2:3], scalar2=None, op0=ALU.mult), reads=[xt, ss], writes=[xs])
                        steps.append(s_xs)

                        def s_tr():
                            for hlf in range(2):
                                for kk in range(4):
                                    k = hlf * 4 + kk
                                    kb.op('pe', lambda e: e.transpose(psum[2 + hlf][:, kk * 128:(kk + 1) * 128], xs[:, k * 128:(k + 1) * 128], identF[:]),
                                          reads=[xs, identF], writes=[psum[2 + hlf]], chain=(kk > 0))
                        steps.append(s_tr)

                        def s_ev():
                            for hlf in range(2):
                                for kk in range(4):
                                    k = hlf * 4 + kk
                                    kb.op('dve', lambda e: e.tensor_scalar(out=h2T[:, k, :], in0=psum[2 + hlf][:, kk * 128:(kk + 1) * 128], scalar1=sc2[:, k:k + 1],
                                                                           scalar2=modT[:, 24 + k, 0:1], op0=ALU.mult, op1=ALU.add), reads=[psum[2 + hlf], modT, sc2], writes=[h2T])
                        steps.append(s_ev)

                        def mk_q(g):
                            def f():
                                wb = wbs[g]
                                for c in range(4):
                                    for k in range(8):
                                        kb.op('pe', lambda e: e.matmul(psum[2 + g % 2][:, c * 128:(c + 1) * 128], lhsT=wb[:, k, c * 128:(c + 1) * 128], rhs=h2T[:, k, :],
                                                                       start=(k == 0), stop=(k == 7)), reads=[wb, h2T], writes=[psum[2 + g % 2]], chain=(k > 0))
                                if g + 1 < 4:
                                    wbs[g + 1] = load_w(wq_d[:, (g + 1) * 512:(g + 2) * 512], 512)
                                if g > 0:
                                    kb.op('dve', lambda e: e.tensor_copy(out=qTp[:, 4 * (g - 1):4 * g, :].rearrange("p m t -> p (m t)"), in_=psum[2 + (g - 1) % 2][:]),
                                          reads=[psum[2 + (g - 1) % 2]], writes=[qTp])
                            return f
                        for g in range(4):
                            steps.append(mk_q(g))

                        def s_q3():
                            kb.op('dve', lambda e: e.tensor_copy(out=qTp[:, 12:16, :].rearrange("p m t -> p (m t)"), in_=psum[3][:]), reads=[psum[3]], writes=[qTp])
                        steps.append(s_q3)

                        def mk_sc(g):
                            def f():
                                pbk = psum[2 + g % 2]
                                if g < 4:
                                    for c in range(4):
                                        ph_ = 4 * g + c
                                        kb.op('pe', lambda e: e.matmul(pbk[:, c * 128:(c + 1) * 128], lhsT=qTp[:, ph_, :], rhs=keysT[:, ph_ * 128:(ph_ + 1) * 128], start=True, stop=True),
                                              reads=[qTp, keysT], writes=[pbk], chain=(c > 0))
                                if g > 0:
                                    pbp = psum[2 + (g - 1) % 2]
                                    kb.op('dve', lambda e: e.tensor_copy(out=sc[:, 4 * (g - 1):4 * g, :].rearrange("p m t -> p (m t)"), in_=pbp[:]), reads=[pbp], writes=[sc])
                            return f
                        for g in range(5):
                            steps.append(mk_sc(g))

                        def mk_top(g):
                            def f():
                                for gg in (g, g + 1):
                                    kb.op('dve', lambda e: e.max(out=tv[:, gg, 0:8], in_=sc[:, gg, :]), reads=[sc], writes=[tv])
                                    kb.op('dve', lambda e: e.match_replace(out=work[:, 0:128], in_to_replace=tv[:, gg, 0:8], in_values=sc[:, gg, :], imm_value=NEG),
                                          reads=[sc, tv], writes=[work])
                                    kb.op('dve', lambda e: e.max(out=tv[:, gg, 8:16], in_=work[:, 0:128]), reads=[work], writes=[tv])
                            return f
                        for g in range(0, 16, 2):
                            steps.append(mk_top(g))

                        def mk_cand(p0):
                            def f():
                                for p in (p0, p0 + 1):
                                    kb.op('dve', lambda e: e.tensor_tensor(out=cand[:], in0=tv[:, 2 * p, :].unsqueeze(2).broadcast_to([128, 16, 16]),
                                                                           in1=tv[:, 2 * p + 1, :].unsqueeze(1).broadcast_to([128, 16, 16]), op=ALU.add), reads=[tv], writes=[cand])
                                    cf = cand[:].rearrange("p a b -> p (a b)")
                                    kb.op('dve', lambda e: e.max(out=c16[:, p, 0:8], in_=cf), reads=[cand], writes=[c16])
                                    kb.op('dve', lambda e: e.match_replace(out=work[:], in_to_replace=c16[:, p, 0:8], in_values=cf, imm_value=NEG), reads=[cand, c16], writes=[work])
                                    kb.op('dve', lambda e: e.max(out=c16[:, p, 8:16], in_=work[:]), reads=[work], writes=[c16])
                            return f
                        for p0 in range(0, 8, 2):
                            steps.append(mk_cand(p0))

                        def s_negm():
                            kb.op('dve', lambda e: e.tensor_scalar(out=st8[:, 0, :], in0=c16[:, :, 0], scalar1=-1.0, scalar2=None, op0=ALU.mult), reads=[c16], writes=[st8])
                            kb.op('dve', lambda e: e.tensor_scalar(out=st8[:, 4, :], in0=c16[:, :, 15], scalar1=-1.0, scalar2=1.0e-6, op0=ALU.mult, op1=ALU.add), reads=[c16], writes=[st8])
                        steps.append(s_negm)

                        def s_z():
                            for p in range(8):
                                kb.op('act', lambda e: e.activation(out=e16[:], in_=c16[:, p, :], func=AF.Exp, bias=st8[:, 0, p:p + 1], accum_out=st8[:, 1, p:p + 1]),
                                      reads=[c16, st8], writes=[e16, st8])
                            kb.op('act', lambda e: e.activation(out=st8[:, 2, :], in_=st8[:, 1, :], func=AF.Ln), reads=[st8], writes=[st8])
                        steps.append(s_z)

                        def s_cb():
                            kb.op('dve', lambda e: e.tensor_tensor(out=st8[:, 3, :], in0=st8[:, 0, :], in1=st8[:, 2, :], op=ALU.subtract), reads=[st8], writes=[st8])
                            kb.op('dve', lambda e: e.tensor_tensor(out=st8[:, 3, :], in0=st8[:, 3, :], in1=st8[:, 4, :], op=ALU.subtract), reads=[st8], writes=[st8])
                        steps.append(s_cb)

                        for f in steps:
                            f()
                            yield

                    def stageB(i):
                        sc, c16, st8 = sc2_[i % 2], c162[i % 2], st82[i % 2]
                        its = [(part, p) for part in range(8) for p in range(8)]
                        nI = len(its)
                        for n in range(nI + 4):
                            if n < nI and its[n][1] == 0:
                                yield its[n][0]
                            m = n - 4
                            if 0 <= m < nI:
                                part, p = its[m]
                                eq = Eq[m % 3]
                                wsl = W[:, part * 2048:(part + 1) * 2048].rearrange("p (a b) -> p a b", b=128)
                                if p > 0:
                                    kb.op('dve', lambda e: e.tensor_tensor(out=wsl, in0=wsl, in1=eq[:], op=ALU.add), reads=[Wb[part], eq], writes=[Wb[part]])
                            m = n - 2
                            if 0 <= m < nI:
                                part, p = its[m]
                                sq, eq = Sq[m % 3], Eq[m % 3]
                                wsl = W[:, part * 2048:(part + 1) * 2048].rearrange("p (a b) -> p a b", b=128)
                                sqf = sq[:].rearrange("p a b -> p (a b)")
                                if m % 4 != 3:
                                    kb.op('act', lambda e: e.activation(out=sqf, in_=sqf, func=AF.Prelu, alpha=1.0e30), reads=[sq], writes=[sq])
                                if p == 0:
                                    kb.op('act', lambda e: e.activation(out=wsl, in_=sq[:], func=AF.Exp, bias=st8[:, 3, p:p + 1]), reads=[sq, st8], writes=[Wb[part]])
                                else:
                                    kb.op('act', lambda e: e.activation(out=eq[:], in_=sq[:], func=AF.Exp, bias=st8[:, 3, p:p + 1]), reads=[sq, st8], writes=[eq])
                            if n < nI:
                                part, p = its[n]
                                sq = Sq[n % 3]
                                kb.op('dve', lambda e: e.scalar_tensor_tensor(out=sq[:], in0=sc[:, 2 * p, part * 16:(part + 1) * 16].unsqueeze(2).broadcast_to([128, 16, 128]),
                                                                              scalar=st8[:, 4, p:p + 1], in1=sc[:, 2 * p + 1, :].unsqueeze(1).broadcast_to([128, 16, 128]),
                                                                              op0=ALU.add, op1=ALU.add), reads=[sc, st8], writes=[sq])
                                if n % 4 == 3:
                                    kb.op('dve', lambda e: e.scalar_tensor_tensor(out=sq[:], in0=sq[:], scalar=1.0e30, in1=sq[:], op0=ALU.mult, op1=ALU.min),
                                          reads=[sq], writes=[sq])
                            yield None

                    def pull(gen, limit_part=None, state=None):
                        if gen is None:
                            return False
                        if state is not None and state.get('pending') is not None:
                            if limit_part is not None and state['pending'] > limit_part:
                                return True
                            state['pending'] = None
                        try:
                            r = next(gen)
                        except StopIteration:
                            return False
                        if r is not None and state is not None:
                            if limit_part is not None and r > limit_part:
                                state['pending'] = r
                        return True

                    def stageC(i, genB, stB, genA, j_from=-3, nxt=None):
                        s3 = i % NB3
                        xt, h2T = xt3[s3], h2T3[s3]
                        hcur = [h2T]
                        tsl = slice(i * 128, (i + 1) * 128)
                        aliveB, aliveA = genB is not None, genA is not None

                        def uload(eg):
                            u_ = ub[eg % 2]
                            kb.dma('sp', u_[:], uTb_d[:, eg * 512:(eg + 1) * 512].rearrange("(k p) n -> p k n", p=128), reads=[uTb_b], writes=[u_])

                        def vload(eg):
                            v_ = vbf[eg % 2]
                            kb.dma('sp', v_[:], vb_d[eg * 512:(eg + 1) * 512, :].rearrange("(c p) n -> p c n", p=128), reads=[vb_b], writes=[v_])

                        def mmA(eg):
                            pa = psum[4 + eg % 2]
                            u_ = ub[eg % 2]
                            for k in range(8):
                                kb.op('pe', lambda e: e.matmul(pa[:], lhsT=hcur[0][:, k, :], rhs=u_[:, k, :], start=(k == 0), stop=(k == 7)), reads=[hcur[0], u_], writes=[pa], chain=(k > 0))

                        def gel(eg):
                            pa = psum[4 + eg % 2]
                            g_ = Gt[eg % 2]
                            kb.op('act', lambda e: e.activation(out=g_[:], in_=pa[:], func=AF.Gelu), reads=[pa], writes=[g_])
                            kb.op('pool', lambda e: e.tensor_tensor(out=Xt[eg % 2][:], in0=g_[:], in1=W[:, eg * 512:(eg + 1) * 512], op=ALU.mult),
                                  reads=[g_, Wb[eg // 4]], writes=[Xt[eg % 2]])

                        def trn(eg):
                            p6 = pbf(6 + eg % 2)
                            for c in range(4):
                                kb.op('pe', lambda e: e.transpose(p6[:, c * 128:(c + 1) * 128], Xt[eg % 2][:, c * 128:(c + 1) * 128], identB[:]),
                                      reads=[Xt[eg % 2], identB], writes=[psum[6 + eg % 2]], chain=(c > 0))

                        def cpy(eg):
                            p6 = pbf(6 + eg % 2)
                            kb.op('act', lambda e: e.copy(out=XT[eg % 2][:].rearrange("p c t -> p (c t)"), in_=p6[:, 0:512]), reads=[psum[6 + eg % 2]], writes=[XT[eg % 2]])

                        def mmV(eg):
                            v_ = vbf[eg % 2]
                            for hf in range(2):
                                for c in range(4):
                                    kb.op('pe', lambda e: e.matmul(psum[hf][:], lhsT=XT[eg % 2][:, c, :], rhs=v_[:, c, hf * 512:(hf + 1) * 512],
                                                                   start=(eg == 0 and c == 0), stop=(eg == NEG_ - 1 and c == 3)), reads=[XT[eg % 2], v_], writes=[psum[hf]], chain=(hf + c > 0))

                        def c_ops(j):
                            if 0 <= j - 1 < NEG_:
                                mmV(j - 1)
                            if 0 <= j + 1 < NEG_:
                                vload(j + 1)
                            if 0 <= j + 3 < NEG_:
                                uload(j + 3)
                            if 0 <= j + 2 < NEG_:
                                mmA(j + 2)
                            if 0 <= j + 1 < NEG_:
                                gel(j + 1)
                            if 0 <= j < NEG_:
                                trn(j)

                        for j in range(j_from, NEG_ + 1):
                            c_ops(j)
                            lim = (j + 2) // 4 - 1
                            for it_ in range(3):
                                if aliveB:
                                    aliveB = pull(genB, lim, stB)
                                    if stB.get('pending') is not None and aliveA:
                                        aliveA = pull(genA)
                                elif aliveA:
                                    aliveA = pull(genA)
                                if it_ == 0 and 0 <= j < NEG_:
                                    cpy(j)
                        if nxt is not None:
                            hcur[0] = nxt
                            for j in range(-3, 0):
                                c_ops(j)
                            hcur[0] = h2T
                        for hf in range(2):
                            kb.op('dve', lambda e: e.tensor_tensor(out=x2[:, hf * 512:(hf + 1) * 512], in0=psum[hf][:], in1=g2rep[:, hf * 512:(hf + 1) * 512], op=ALU.mult),
                                  reads=[psum[hf], g2rep], writes=[x2])
                        kb.op('pool', lambda e: e.tensor_tensor(out=x2[:], in0=x2[:], in1=xt[:], op=ALU.add), reads=[x2, xt], writes=[x2])
                        ss = ss2[1]
                        kb.op('act', lambda e: e.activation(out=junk[:], in_=x2[:], func=AF.Square, accum_out=ss[:, 0:1]), reads=[x2], writes=[junk, ss])
                        kb.op('act', lambda e: e.activation(out=ss[:, 1:2], in_=ss[:, 0:1], func=AF.Ln, scale=1.0 / D, bias=epsc[:, 0:1]), reads=[ss, epsc], writes=[ss])
                        kb.op('act', lambda e: e.activation(out=ss[:, 2:3], in_=ss[:, 1:2], func=AF.Exp, scale=-0.5), reads=[ss], writes=[ss])
                        kb.op('dve', lambda e: e.scalar_tensor_tensor(out=x2[:], in0=x2[:], scalar=ss[:, 2:3], in1=nfrep[:], op0=ALU.mult, op1=ALU.mult),
                              reads=[x2, ss, nfrep], writes=[x2])
                        kb.dma('sp', y_d[b, tsl, :], x2[:], reads=[x2], writes=[y_b])
                        while aliveB:
                            aliveB = pull(genB, 99, stB)
                        while aliveA:
                            aliveA = pull(genA)

                    for _ in stageA(0):
                        pass
                    if NL > 1:
                        for _ in stageA(1):
                            pass
                    for _ in stageB(0):
                        pass
                    for i in range(NL):
                        gB = stageB(i + 1) if i + 1 < NL else None
                        gA = stageA(i + 2) if i + 2 < NL else None
                        stageC(i, gB, {'pending': None}, gA, j_from=(-3 if i == 0 else 0), nxt=(h2T3[(i + 1) % NB3] if i + 1 < NL else None))
                kb.barrier([y_b])
        kb.wait_all('sp', [y_b] + [x1_b[b][jl] for b in range(NSEQ) for jl in range(NL)])
    return nc


_CACHE = {}


def _prep_shared(inp):
    f = np.float32
    w_in = inp["w_in"][0]
    mq, mk, mv, mo = w_in[:, 0:512], w_in[:, 512:1024], w_in[:, 1024:1536], w_in[:, 1536:2048]
    mg = w_in[:, 2048:2064]
    gq, gk, gv, gr = w_in[:, 2064:2320], w_in[:, 2320:2576], w_in[:, 2576:3088], w_in[:, 3088:3600]
    glr = w_in[:, 3600:3632]
    glrpad = np.zeros((D, 128), f)
    glrpad[:, 0:16] = glr[:, 0:16]
    glrpad[:, 32:48] = glr[:, 16:32]
    lrw2 = np.zeros((64, 256), f)
    lrw2[0:16] = inp["gla_lr_w2"][0, 0]
    lrw2[32:48] = inp["gla_lr_w2"][0, 1]
    sh = {
        "w_mod": np.ascontiguousarray(inp["w_mod"][0]),
        "bmodT": np.ascontiguousarray(inp["b_mod"][0].reshape(48, 128).T),
        "b_mod": np.ascontiguousarray(inp["b_mod"][0].reshape(1, -1)),
        "ngT": np.ascontiguousarray(np.stack([inp["norm1_g"][0].reshape(8, 128).T, inp["norm2_g"][0].reshape(8, 128).T], axis=1)),
        "w_fm": np.ascontiguousarray(np.concatenate([mq, mk, gq, gk, glrpad], axis=1)),
        "w_tm": np.ascontiguousarray(np.concatenate([mv, mo, gv, gr, mg], axis=1)),
        "cwT": np.ascontiguousarray(inp["ml_conv_w"][0].T.reshape(8, 128, 3).transpose(1, 0, 2)),
        "gate_b": np.ascontiguousarray(inp["ml_gate_b"][0].reshape(1, 16)),
        "ml_g": np.ascontiguousarray(inp["ml_norm_g"][0].reshape(1, 512)),
        "gla_g": np.ascontiguousarray(inp["gla_norm_g"][0].reshape(1, 512)),
        "lrw2": lrw2,
        "alpha_b": np.ascontiguousarray(inp["gla_alpha_b"][0].reshape(1, 512)),
        "w_out": np.ascontiguousarray(inp["w_out"][0]),
        "wq": np.ascontiguousarray(inp["peer_wq"][0]),
        "keysT": np.ascontiguousarray(inp["peer_keys"][0].transpose(3, 0, 1, 2).reshape(128, 2048)),
        "uT": np.ascontiguousarray(inp["peer_u"][0].T),
        "v": np.ascontiguousarray(inp["peer_v"][0]),
        "nf_g": np.ascontiguousarray(inp["norm_f_g"].reshape(1, D)),
    }
    return {k: np.asarray(v, dtype=f) for k, v in sh.items()}


def run(inp, n_cores, dbg=False):
    inp = {k: np.asarray(v) for k, v in inp.items()}
    B, N, _ = inp["x"].shape
    C = inp["ctx"].shape[1]
    NSEQ = B // n_cores
    key = (NSEQ, N, C, dbg)
    if key not in _CACHE:
        _CACHE[key] = build(NSEQ, N, C, dbg)
    nc = _CACHE[key]
    sh = _prep_shared(inp)
    in_maps = []
    for i in range(n_cores):
        sl = slice(i * NSEQ, (i + 1) * NSEQ)
        cc = np.concatenate([inp["c"][sl], inp["c_ctx"][None, :]], axis=0).astype(np.float32)
        m = dict(sh)
        m["x"] = np.ascontiguousarray(inp["x"][sl], dtype=np.float32)
        m["ctx"] = np.ascontiguousarray(inp["ctx"][sl], dtype=np.float32)
        m["ccT"] = np.ascontiguousarray(cc.reshape(NSEQ + 1, 8, 128).transpose(2, 1, 0))
        in_maps.append(m)
    res = run_bass_kernel_spmd(nc, in_maps, core_ids=list(range(n_cores)))
    y = np.concatenate([r["y"] for r in res.results], axis=0)
    if dbg:
        return y, np.concatenate([r["x1s"] for r in res.results], axis=0)
    return y


def kernel(**inputs):
    return run(inputs, 8).astype(np.float32)
```
